# Optimizing a Trainium2 kernel written in Bass

```python
import math
import jax, jax.numpy as jnp
from jax import lax
import numpy as np

D_MODEL = 1024
BATCH = 4
SEQ = 4096
DEPTH = 1

D_MIX = D_MODEL
N_ATTN_HEADS = 8
ATTN_HEAD_DIM = 64
D_ATTN = N_ATTN_HEADS * ATTN_HEAD_DIM
D_POOL = D_MIX - D_ATTN
POOL_WINDOWS = (2, 4, 8, 16)
N_POOL_GROUPS = len(POOL_WINDOWS)
POOL_GROUP_DIM = D_POOL // N_POOL_GROUPS
D_IN = 3 * D_ATTN + D_POOL
MOBA_BLOCK = 256
MOBA_TOPK = 3
Q_CHUNK = 32
N_MEM = 256
XA_HEADS = 4
XA_HEAD_DIM = D_MODEL // XA_HEADS
D_FF = 2816
MACARON_WEIGHT = 0.5
RMS_EPS = 1e-6

kernel_name = "hybrid_moba_pool_macaron_block"


def rmsnorm(x, g):
    xf = x.astype(jnp.float32)
    r = lax.rsqrt(jnp.mean(xf * xf, axis=-1, keepdims=True) + RMS_EPS)
    return (xf * r).astype(x.dtype) * g


def swiglu(h, w_gate, w_up, w_down):
    return (jax.nn.silu(h @ w_gate) * (h @ w_up)) @ w_down


def alibi_slopes(n_heads):
    s = 2.0 ** (-8.0 * np.arange(1, n_heads + 1) / n_heads)
    return jnp.asarray(s, dtype=jnp.float32)


def moba_attention(q, k, v, slopes):
    B, H, S, dh = q.shape
    L = MOBA_BLOCK
    s_pad = ((S + L - 1) // L) * L
    pad = s_pad - S
    kp = jnp.pad(k, ((0, 0), (0, 0), (0, pad), (0, 0)))
    vp = jnp.pad(v, ((0, 0), (0, 0), (0, pad), (0, 0)))
    nb = s_pad // L
    n_sel = min(MOBA_TOPK, nb)
    kb = kp.reshape(B, H, nb, L, dh)
    vb = vp.reshape(B, H, nb, L, dh)
    k_mean = jnp.mean(kb, axis=3)
    qs = q * (dh ** -0.5)
    bi = jnp.arange(B)[:, None, None, None]
    hi = jnp.arange(H)[None, :, None, None]
    lpos = jnp.arange(L)
    slot = jnp.arange(n_sel)
    blk_ids = jnp.arange(nb)
    n_chunks = S // Q_CHUNK

    def chunk(c):
        start = c * Q_CHUNK
        qc = lax.dynamic_slice_in_dim(qs, start, Q_CHUNK, axis=2)
        qpos = start + jnp.arange(Q_CHUNK)
        qpos_f = qpos.astype(jnp.float32)
        own = start // L
        gate = jnp.einsum('bhqd,bhnd->bhqn', qc, k_mean).astype(jnp.float32)
        gate = jnp.where(blk_ids < own, gate, -jnp.inf)
        _, idx = lax.top_k(gate, n_sel)
        slot_ok = slot < own
        k_sel = kb[bi, hi, idx]
        v_sel = vb[bi, hi, idx]
        kpos_sel = (idx[..., None] * L + lpos).astype(jnp.float32)
        s_sel = (jnp.einsum('bhqd,bhqrld->bhqrl', qc, k_sel).astype(jnp.float32)
                 - slopes[:, None, None, None] * (qpos_f[:, None, None] - kpos_sel))
        s_sel = jnp.where(slot_ok[:, None], s_sel, -jnp.inf)
        k_own = lax.dynamic_index_in_dim(kb, own, axis=2, keepdims=False)
        v_own = lax.dynamic_index_in_dim(vb, own, axis=2, keepdims=False)
        kpos_own = own * L + lpos
        s_own = (jnp.einsum('bhqd,bhld->bhql', qc, k_own).astype(jnp.float32)
                 - slopes[:, None, None] * (qpos_f[:, None] - kpos_own.astype(jnp.float32)[None, :]))
        s_own = jnp.where(kpos_own[None, :] <= qpos[:, None], s_own, -jnp.inf)
        scores = jnp.concatenate([s_sel.reshape(B, H, Q_CHUNK, n_sel * L), s_own], axis=-1)
        p = jax.nn.softmax(scores, axis=-1)
        p_sel = p[..., :n_sel * L].reshape(B, H, Q_CHUNK, n_sel, L).astype(v.dtype)
        p_own = p[..., n_sel * L:].astype(v.dtype)
        return (jnp.einsum('bhqrl,bhqrld->bhqd', p_sel, v_sel)
                + jnp.einsum('bhql,bhld->bhqd', p_own, v_own))

    outs = lax.map(chunk, jnp.arange(n_chunks))
    return outs.transpose(1, 2, 0, 3, 4).reshape(B, H, S, dh)


def multiscale_pool(p, pool_w, pool_scale):
    B, S, C = p.shape
    pf = p.reshape(B, S, N_POOL_GROUPS, POOL_GROUP_DIM).astype(jnp.float32)
    cs = jnp.concatenate([jnp.zeros((B, 1, N_POOL_GROUPS, POOL_GROUP_DIM), jnp.float32),
                          lax.cumsum(pf, axis=1)], axis=1)
    t = jnp.arange(S)
    outs = []
    for g, w in enumerate(POOL_WINDOWS):
        lo = jnp.maximum(t + 1 - w, 0)
        cnt = (t + 1 - lo).astype(jnp.float32)
        mean = (cs[:, 1:, g] - cs[:, lo, g]) / cnt[None, :, None]
        outs.append(mean - pf[:, :, g])
    d = jnp.stack(outs, axis=2).astype(p.dtype)
    y = jnp.einsum('bsgc,gcd->bsgd', d, pool_w)
    return y.reshape(B, S, C) * pool_scale


def memory_cross_attention(h, mem_n, wq, wkv, wo):
    B, S, _ = h.shape
    M = mem_n.shape[1]
    q = (h @ wq).reshape(B, S, XA_HEADS, XA_HEAD_DIM)
    kv = mem_n @ wkv
    k = kv[..., :D_MODEL].reshape(B, M, XA_HEADS, XA_HEAD_DIM)
    v = kv[..., D_MODEL:].reshape(B, M, XA_HEADS, XA_HEAD_DIM)
    s = jnp.einsum('bshd,bmhd->bhsm', q, k).astype(jnp.float32) * (XA_HEAD_DIM ** -0.5)
    p = jax.nn.softmax(s, axis=-1).astype(v.dtype)
    o = jnp.einsum('bhsm,bmhd->bshd', p, v).reshape(B, S, XA_HEADS * XA_HEAD_DIM)
    return o @ wo


def setup_inputs(seed: int = 0) -> dict:
    key = jax.random.key(seed)
    ks = jax.random.split(key, 24)

    def nrm(k, shape, fan_in):
        return jax.random.normal(k, shape, jnp.float32) * fan_in ** -0.5

    def gain(k, shape):
        return 1.0 + 0.1 * jax.random.normal(k, shape, jnp.float32)

    L = DEPTH
    return {
        "x": jax.random.normal(ks[0], (BATCH, SEQ, D_MODEL), jnp.float32),
        "mem": jax.random.normal(ks[1], (BATCH, N_MEM, D_MODEL), jnp.float32),
        "ffn1_pre_g": gain(ks[2], (L, D_MODEL)),
        "ffn1_w_gate": nrm(ks[3], (L, D_MODEL, D_FF), D_MODEL),
        "ffn1_w_up": nrm(ks[4], (L, D_MODEL, D_FF), D_MODEL),
        "ffn1_w_down": nrm(ks[5], (L, D_FF, D_MODEL), D_FF),
        "ffn1_post_g": gain(ks[6], (L, D_MODEL)),
        "mix_pre_g": gain(ks[7], (L, D_MODEL)),
        "w_in": nrm(ks[8], (L, D_MODEL, D_IN), D_MODEL),
        "pool_w": nrm(ks[9], (L, N_POOL_GROUPS, POOL_GROUP_DIM, POOL_GROUP_DIM), POOL_GROUP_DIM),
        "pool_scale": gain(ks[10], (L, D_POOL)),
        "w_out": nrm(ks[11], (L, D_MIX, D_MODEL), D_MIX),
        "mix_post_g": gain(ks[12], (L, D_MODEL)),
        "xa_pre_g": gain(ks[13], (L, D_MODEL)),
        "mem_g": gain(ks[14], (L, D_MODEL)),
        "xa_wq": nrm(ks[15], (L, D_MODEL, XA_HEADS * XA_HEAD_DIM), D_MODEL),
        "xa_wkv": nrm(ks[16], (L, D_MODEL, 2 * XA_HEADS * XA_HEAD_DIM), D_MODEL),
        "xa_wo": nrm(ks[17], (L, XA_HEADS * XA_HEAD_DIM, D_MODEL), XA_HEADS * XA_HEAD_DIM),
        "xa_post_g": gain(ks[18], (L, D_MODEL)),
        "ffn2_pre_g": gain(ks[19], (L, D_MODEL)),
        "ffn2_w_gate": nrm(ks[20], (L, D_MODEL, D_FF), D_MODEL),
        "ffn2_w_up": nrm(ks[21], (L, D_MODEL, D_FF), D_MODEL),
        "ffn2_w_down": nrm(ks[22], (L, D_FF, D_MODEL), D_FF),
        "ffn2_post_g": gain(ks[23], (L, D_MODEL)),
    }


def reference(x, mem, ffn1_pre_g, ffn1_w_gate, ffn1_w_up, ffn1_w_down, ffn1_post_g,
              mix_pre_g, w_in, pool_w, pool_scale, w_out, mix_post_g,
              xa_pre_g, mem_g, xa_wq, xa_wkv, xa_wo, xa_post_g,
              ffn2_pre_g, ffn2_w_gate, ffn2_w_up, ffn2_w_down, ffn2_post_g):
    B, S, _ = x.shape
    slopes = alibi_slopes(N_ATTN_HEADS)
    for l in range(DEPTH):
        h = rmsnorm(x, ffn1_pre_g[l])
        f = swiglu(h, ffn1_w_gate[l], ffn1_w_up[l], ffn1_w_down[l])
        x = x + MACARON_WEIGHT * rmsnorm(f, ffn1_post_g[l])

        h = rmsnorm(x, mix_pre_g[l])
        proj = h @ w_in[l]
        q = proj[..., :D_ATTN]
        k = proj[..., D_ATTN:2 * D_ATTN]
        v = proj[..., 2 * D_ATTN:3 * D_ATTN]
        p_in = proj[..., 3 * D_ATTN:]
        to_heads = lambda t: t.reshape(B, S, N_ATTN_HEADS, ATTN_HEAD_DIM).transpose(0, 2, 1, 3)
        attn = moba_attention(to_heads(q), to_heads(k), to_heads(v), slopes)
        attn = attn.transpose(0, 2, 1, 3).reshape(B, S, D_ATTN)
        pool = multiscale_pool(p_in, pool_w[l], pool_scale[l])
        y = jnp.concatenate([attn, pool], axis=-1) @ w_out[l]
        x = x + rmsnorm(y, mix_post_g[l])

        h = rmsnorm(x, xa_pre_g[l])
        mem_n = rmsnorm(mem, mem_g[l])
        c = memory_cross_attention(h, mem_n, xa_wq[l], xa_wkv[l], xa_wo[l])
        x = x + rmsnorm(c, xa_post_g[l])

        h = rmsnorm(x, ffn2_pre_g[l])
        f = swiglu(h, ffn2_w_gate[l], ffn2_w_up[l], ffn2_w_down[l])
        x = x + MACARON_WEIGHT * rmsnorm(f, ffn2_post_g[l])
    return x
```

```python
from contextlib import ExitStack
import numpy as np
import concourse.bass as bass
import concourse.mybir as mybir
from concourse.bass_utils import run_bass_kernel_spmd

F32 = mybir.dt.float32
BF16 = mybir.dt.bfloat16
ALU = mybir.AluOpType
ACT = mybir.ActivationFunctionType
AX = mybir.AxisListType

D = 1024
SEQ = 4096
NBATCH = 4
DFF = 2816
NFC = DFF // 128
TT = 512
NH = 8
NEG = -30000.0
EPS = 1e-6
NSLOT = 8


class Buf:
    __slots__ = ("name", "last_write", "reads")

    def __init__(self, name):
        self.name = name
        self.last_write = None
        self.reads = []


class EngState:
    def __init__(self, name):
        self.name = name
        self.count = 0
        self.items = []
        self.seen = {}


class Sched:
    ENG = ("tensor", "vector", "scalar", "gpsimd", "sync")

    def __init__(self, nc):
        self.nc = nc
        self.es = ExitStack()
        self.eng = {n: EngState(n) for n in self.ENG}
        self.sems = {}
        for n in self.ENG:
            self.sems[("eng", n)] = self.es.enter_context(nc.semaphore("sem_" + n))
        self.chan_count = {}

    def chan(self, name):
        key = ("chan", name)
        if key not in self.sems:
            self.sems[key] = self.es.enter_context(self.nc.semaphore("ch_" + name))
            self.chan_count[key] = 0
        return key

    def _wait(self, e, tok):
        key, val = tok
        if key == ("eng", e.name) and val > e.count:
            return
        if e.seen.get(key, 0) >= val:
            return
        e.seen[key] = val
        e.items.append(("wait", key, val))

    def _deps(self, e, reads, writes):
        own = ("eng", e.name)
        skip_own = e.name == "tensor"
        for b in reads:
            t = b.last_write
            if t is not None and not (skip_own and t[0] == own):
                self._wait(e, t)
        for b in writes:
            t = b.last_write
            if t is not None and not (skip_own and t[0] == own):
                self._wait(e, t)
            for r in b.reads:
                if not (skip_own and r[0] == own):
                    self._wait(e, r)

    @staticmethod
    def _mark(tok, reads, writes):
        for b in writes:
            b.last_write = tok
            b.reads = []
        for b in reads:
            rl = b.reads
            if rl and rl[-1][0] == tok[0]:
                rl[-1] = tok
            else:
                rl.append(tok)

    def op(self, engname, fn, reads=(), writes=(), signal=True):
        e = self.eng[engname]
        self._deps(e, reads, writes)
        tok = (("eng", engname), e.count + 1)
        if signal:
            e.count += 1
        e.items.append(("op", fn, signal))
        self._mark(tok, reads, writes)
        return tok

    def dma(self, queue, chan, fn, reads=(), writes=()):
        e = self.eng[queue]
        key = self.chan(chan)
        if self.chan_count[key] > 0:
            self._wait(e, (key, self.chan_count[key]))
        self._deps(e, reads, writes)
        self.chan_count[key] += 16
        tok = (key, self.chan_count[key])
        e.items.append(("dma", fn, key))
        self._mark(tok, reads, writes)
        return tok

    def final_wait(self, engname, toks):
        e = self.eng[engname]
        for t in toks:
            self._wait(e, t)

    def emit(self):
        nc = self.nc
        sems = self.sems

        def run(e, engobj):
            own = sems[("eng", e.name)]
            for it in e.items:
                if it[0] == "wait":
                    engobj.wait_ge(sems[it[1]], it[2])
                elif it[0] == "op":
                    ins = it[1](engobj)
                    if it[2]:
                        ins.then_inc(own, 1)
                else:
                    it[1](engobj).then_inc(sems[it[2]], 16)

        with nc.allow_low_precision("bf16 matmul operands, fp32 accumulation by design"), nc.Block() as block:
            @block.tensor
            def _(t):
                run(self.eng["tensor"], t)

            @block.vector
            def _(v):
                run(self.eng["vector"], v)

            @block.scalar
            def _(s):
                run(self.eng["scalar"], s)

            @block.gpsimd
            def _(g):
                run(self.eng["gpsimd"], g)

            @block.sync
            def _(sy):
                run(self.eng["sync"], sy)
        self.es.close()


WFAMS = {
    "g1": (NFC, 128, 1024), "u1": (NFC, 128, 1024), "d1": (NFC, 128, 1024),
    "g2": (NFC, 128, 1024), "u2": (NFC, 128, 1024), "d2": (NFC, 128, 1024),
    "wq": (4, 128, 1024), "wk": (4, 128, 1024), "wv": (4, 128, 1024), "wp": (4, 128, 1024),
    "woa": (8, 64, 1024), "wop": (8, 128, 512),
    "xq": (8, 128, 1024), "xk": (8, 128, 1024), "xv": (8, 128, 1024), "xo": (8, 128, 1024),
    "pw": (1, 128, 512),
}
CONV_ORDER = ["g1", "u1", "d1", "wk", "wv", "wq", "wp", "pw", "woa", "wop", "xk", "xv", "xq", "xo",
              "g2", "u2", "d2"]
G_F1PRE, G_F1POST, G_MIXPRE, G_MIXPOST, G_XAPRE, G_MEM, G_XAPOST, G_F2PRE, G_F2POST, G_PSC = [8 * i for i in range(10)]
NGCOL = 76


class _Stop(Exception):
    pass


def build_program(stage=None):
    nc = bass.Bass("TRN2", target_bir_lowering=False)
    S = Sched(nc)
    dbg = nc.dram_tensor("dbg", [128, 8 * TT], F32, kind="ExternalOutput").ap() if stage else None
    dbg_toks = []

    def checkpoint(name, items):
        if stage != name:
            return
        off = 0
        for i, (ap, bufs, P, n) in enumerate(items):
            dbg_toks.append(S.dma("gpsimd", "dbg%d" % (i % 4), lambda e, ap=ap, off=off, P=P, n=n: e.dma_start(out=dbg[0:P, off:off + n], in_=ap), reads=bufs))
            off += n
        raise _Stop()

    def din(name, shape, dt=F32):
        return nc.dram_tensor(name, list(shape), dt, kind="ExternalInput").ap()

    xT = din("xT", [8, 128, 8, TT])
    memT = din("memT", [128, 8, 256])
    gains_d = din("gains", [128, NGCOL])
    albias_d = din("albias", [128, 640])
    vb_d = din("vb", [128, 8 * 128])
    tri_d = din("tri", [128, 128])
    identn_d = din("identn", [128, 128])
    qxc_d = din("qxc", [2, NH * TT])
    kx_d = din("kx", [18, 16 * 128])
    inv0_d = din("inv0", [128, 64])
    hsel_d = din("hsel", [128, 8])
    outT = nc.dram_tensor("outT", [4, 128, 8, TT], F32, kind="ExternalOutput").ap()
    w_in_d, w_sc_d, w_sc_buf = {}, {}, {}
    for fam, (n, P, X) in WFAMS.items():
        w_in_d[fam] = din("w_" + fam, [n, P, X])
        w_sc_d[fam] = nc.dram_tensor("sc_" + fam, [n, P, X], BF16, kind="Internal").ap()
        w_sc_buf[fam] = [Buf("sc_%s_%d" % (fam, c)) for c in range(n)]

    A = nc.alloc_sbuf_tensor
    KT = A("KT", [128, 4, SEQ], BF16)
    VA = A("VA", [128, 32, NH, 65], BF16)
    KX = A("KX", [18, 16, 128], BF16)
    QX = A("QX", [18, NH, TT], BF16)
    XS = [A("XS%d" % i, [128, 8, TT], F32) for i in range(2)]
    FB = A("FB", [128, 8, 528], F32)
    SCR = A("SCR", [128, 30, TT], BF16)
    WS = A("WS", [128, NSLOT, 1024], BF16)
    KMX = A("KMX", [128, 8, 256], BF16)
    VMX = A("VMX", [128, 2, 1024], BF16)
    RT = A("RT", [128, TT], F32)
    TMP = A("TMP", [128, 2, TT], F32)
    GA = A("GA", [128, NGCOL], F32)
    ALB = A("ALB", [128, 640], F32)
    VBT = A("VBT", [128, 8, 128], F32)
    TRI = A("TRI", [128, 128], F32)
    IDN = A("IDN", [128, 128], BF16)
    ONES = A("ONES", [128, 128], BF16)
    EPST = A("EPST", [128, 1], F32)
    PW = A("PW", [128, 4, 128], BF16)
    KMS = A("KMS", [128, 4, 16], F32)
    KMB = A("KMB", [128, 4, 16], BF16)
    GT = A("GT", [128, 128], F32)
    T8 = A("T8", [128, NH, 8], F32)
    THR = A("THR", [128, NH], F32)
    MSEL = A("MSEL", [128, 128], BF16)
    RR = A("RR", [128, TT], BF16)
    BCS = A("BCS", [64, TT], F32)
    HALO = [A("HALO%d" % i, [128, 4, 16], F32) for i in range(2)]
    HUSE = A("HUSE", [128, 4, 16], F32)
    INV0 = A("INV0", [128, 4, 16], F32)
    HSEL = A("HSEL", [128, 8], F32)
    D16 = A("D16", [128, 16], F32)
    PSB = [nc.alloc_psum_tensor("PS%d" % i, [128, TT], F32) for i in range(8)]

    bKT = [[Buf("KT%d_%d" % (pr, T)) for T in range(8)] for pr in range(4)]
    bVA = [Buf("VA%d" % kt) for kt in range(32)]
    bVAones = Buf("VAones")
    bKX, bQXc = Buf("KX"), Buf("QXc")
    bQXm = [Buf("QXm%d" % h) for h in range(NH)]
    bXS = [[Buf("XS%d_%d" % (i, k)) for k in range(8)] for i in range(2)]
    bFB = [Buf("FB%d" % k) for k in range(8)]
    bSCR = [Buf("SCR%d" % i) for i in range(30)]
    bWS = [Buf("WS%d" % i) for i in range(NSLOT)]
    bKMX, bVMX = [Buf("KMX%d" % i) for i in range(8)], [Buf("VMX%d" % i) for i in range(2)]
    bRT, bTMP = Buf("RT"), [Buf("TMP0"), Buf("TMP1")]
    bGA, bALB, bVBT, bTRI, bIDN, bONES, bEPS, bPW = (Buf(n) for n in "GA ALB VBT TRI IDN ONES EPS PW".split())
    bKMS, bKMB, bGT, bT8, bTHR, bMSEL, bRR, bBCS = (Buf(n) for n in "KMS KMB GT T8 THR MSEL RR BCS".split())
    bHALO, bHUSE, bINV0, bHSEL, bD16 = [Buf("HALO0"), Buf("HALO1")], Buf("HUSE"), Buf("INV0"), Buf("HSEL"), Buf("D16")
    bPS = [Buf("PS%d" % i) for i in range(8)]

    def scr(i):
        return SCR[:, i, :]

    def vop(fn, reads, writes, signal=True):
        S.op("vector", fn, reads, writes, signal=signal)

    def aop(fn, reads, writes):
        S.op("scalar", fn, reads, writes)

    def mm(out, lhsT, rhs, start, stop, reads, wbuf, sig=False, nosig=False):
        S.op("tensor", lambda e, out=out, lhsT=lhsT, rhs=rhs, start=start, stop=stop:
             e.matmul(out, lhsT=lhsT, rhs=rhs, start=start, stop=stop),
             reads, [wbuf], signal=((stop or sig) and not nosig))

    cload = [0]

    def ld_const(dst, src, buf, queue="sync"):
        cload[0] += 1
        S.dma(queue, ("c%d" if queue == "sync" else "cg%d") % (cload[0] % 4), lambda e, dst=dst, src=src: e.dma_start(out=dst, in_=src), writes=[buf])

    S.op("gpsimd", lambda e: e.memset(ONES[:, :], 1.0), writes=[bONES])
    S.op("gpsimd", lambda e: e.memset(EPST[:, :], EPS), writes=[bEPS])
    S.op("gpsimd", lambda e: e.memset(RR[:, :], 0.0), writes=[bRR])
    S.op("gpsimd", lambda e: e.memset(KMB[:, :, :], 0.0), writes=[bKMB])
    S.op("gpsimd", lambda e: e.memset(VA[:, :, :, 64:65], 1.0), writes=[bVAones])
    S.op("gpsimd", lambda e: e.memset(HALO[0][:, :, :], 0.0), writes=[bHALO[0]])
    S.op("gpsimd", lambda e: e.memset(HALO[1][:, :, :], 0.0), writes=[bHALO[1]])
    ld_const(GA[:, :], gains_d, bGA)
    ld_const(ALB[:, :], albias_d, bALB)
    ld_const(VBT[:, :, :], vb_d.rearrange("p (a b) -> p a b", a=8), bVBT)
    ld_const(TRI[:, :], tri_d, bTRI)
    ld_const(INV0[:, :, :], inv0_d.rearrange("p (a b) -> p a b", a=4), bINV0)
    ld_const(HSEL[:, :], hsel_d, bHSEL)
    ld_const(IDN[:, :], identn_d, bIDN, "gpsimd")
    ld_const(QX[16:18, :, :], qxc_d.rearrange("p (a b) -> p a b", a=NH), bQXc, "gpsimd")
    ld_const(KX[:, :, :], kx_d.rearrange("p (a b) -> p a b", a=16), bKX, "gpsimd")
    vop(lambda e: e.tensor_scalar(out=GA[:, G_F1POST:G_F1POST + 8], in0=GA[:, G_F1POST:G_F1POST + 8], scalar1=0.5, scalar2=None, op0=ALU.mult), [bGA], [bGA])
    vop(lambda e: e.tensor_scalar(out=GA[:, G_F2POST:G_F2POST + 8], in0=GA[:, G_F2POST:G_F2POST + 8], scalar1=0.5, scalar2=None, op0=ALU.mult), [bGA], [bGA])

    cv = [0]
    for fam in CONV_ORDER:
        n = WFAMS[fam][0]
        for c in range(n):
            cv[0] += 1
            S.dma("gpsimd", "cv%d" % (cv[0] % 8),
                  lambda e, fam=fam, c=c: e.dma_start(out=w_sc_d[fam][c], in_=w_in_d[fam][c]),
                  writes=[w_sc_buf[fam][c]])

    wring = [0]

    def wget(fam, c):
        n, P, X = WFAMS[fam]
        s = wring[0] % NSLOT
        wring[0] += 1
        S.dma("sync", "ws%d" % s,
              lambda e, fam=fam, c=c, s=s, P=P, X=X: e.dma_start(out=WS[0:P, s, 0:X], in_=w_sc_d[fam][c]),
              reads=[w_sc_buf[fam][c]], writes=[bWS[s]])
        return s, bWS[s]

    def wk3(s):
        return WS[:, s, :].rearrange("p (k f) -> p k f", k=8)

    def norm_stats(sq_tiles):
        for k, t in enumerate(sq_tiles):
            mm(PSB[6][:, :], ONES[:, :], scr(t), k == 0, k == 7, [bONES, bSCR[t]], bPS[6])
        aop(lambda e: e.activation(out=TMP[:, 0, :], in_=PSB[6][:, :], func=ACT.Sqrt, bias=EPST[:, 0:1], scale=1.0 / D),
            [bPS[6], bEPS], [bTMP[0]])
        vop(lambda e: e.reciprocal(out=RT[:, :], in_=TMP[:, 0, :]), [bTMP[0]], [bRT])

    def prenorm(xs, gcol, ntok=TT, sq_base=8):
        X = XS[xs]
        for k in range(8):
            aop(lambda e, k=k: e.activation(out=scr(sq_base + k), in_=X[:, k, :], func=ACT.Square),
                [bXS[xs][k]], [bSCR[sq_base + k]])
        norm_stats([sq_base + k for k in range(8)])
        for k in range(8):
            vop(lambda e, k=k: e.scalar_tensor_tensor(out=scr(k), in0=X[:, k, :], scalar=GA[:, gcol + k:gcol + k + 1],
                                                      in1=RT[:, :], op0=ALU.mult, op1=ALU.mult),
                [bXS[xs][k], bGA, bRT], [bSCR[k]])

    def evac_for_postnorm(m, ps_i, sq_base):
        vop(lambda e, m=m, ps_i=ps_i: e.tensor_copy(out=FB[:, m, 0:TT], in_=PSB[ps_i][:, :]), [bPS[ps_i]], [bFB[m]])
        aop(lambda e, m=m: e.activation(out=scr(sq_base + m), in_=FB[:, m, 0:TT], func=ACT.Square),
            [bFB[m]], [bSCR[sq_base + m]])

    def postnorm_residual(xs, gcol, sq_base):
        X = XS[xs]
        norm_stats([sq_base + m for m in range(8)])
        for m in range(8):
            vop(lambda e, m=m: e.scalar_tensor_tensor(out=FB[:, m, 0:TT], in0=FB[:, m, 0:TT], scalar=GA[:, gcol + m:gcol + m + 1],
                                                      in1=RT[:, :], op0=ALU.mult, op1=ALU.mult),
                [bFB[m], bGA, bRT], [bFB[m]])
            vop(lambda e, m=m: e.tensor_tensor(out=X[:, m, :], in0=X[:, m, :], in1=FB[:, m, 0:TT], op=ALU.add),
                [bXS[xs][m], bFB[m]], [bXS[xs][m]])

    def ffn(xs, fg, fu, fd, gpre, gpost, mid_hook=None):
        prenorm(xs, gpre)
        for c in range(NFC):
            if c == 6 and mid_hook is not None:
                mid_hook()
            sg, bg = wget(fg, c)
            su, bu = wget(fu, c)
            pg, pu = c % 2, 2 + c % 2
            wg3, wu3 = wk3(sg), wk3(su)
            for k in range(8):
                mm(PSB[pg][:, :], wg3[:, k, :], scr(k), k == 0, k == 7, [bg, bSCR[k]], bPS[pg])
            for k in range(8):
                mm(PSB[pu][:, :], wu3[:, k, :], scr(k), k == 0, k == 7, [bu, bSCR[k]], bPS[pu])
            aop(lambda e, c=c, pg=pg: e.activation(out=TMP[:, c % 2, :], in_=PSB[pg][:, :], func=ACT.Silu),
                [bPS[pg]], [bTMP[c % 2]])
            vop(lambda e, c=c, pu=pu: e.tensor_tensor(out=scr(8 + c), in0=PSB[pu][:, :], in1=TMP[:, c % 2, :], op=ALU.mult),
                [bPS[pu], bTMP[c % 2]], [bSCR[8 + c]])
            if c == 0:
                checkpoint("ffn_a0", [(scr(8), [bSCR[8]], 128, TT)])
            if c == 3:
                checkpoint("ffn_a3", [(scr(8 + i), [bSCR[8 + i]], 128, TT) for i in range(4)])
            checkpoint("ffn_c%d" % c, [(scr(8 + i), [bSCR[8 + i]], 128, TT) for i in range(max(0, c - 3), c + 1)])
        checkpoint("ffn_a", [(scr(8 + i), [bSCR[8 + i]], 128, TT) for i in range(14, 22)])
        import os
        MLIM = int(os.environ.get("MLIM", "8"))
        for c in range(NFC):
            sd, bd = wget(fd, c)
            for m in range(MLIM):
                mm(PSB[m][:, :], WS[:, sd, m * 128:(m + 1) * 128], scr(8 + c), c == 0, c == NFC - 1,
                   [bd, bSCR[8 + c]], bPS[m], sig=(m == MLIM - 1))
        checkpoint("ffn_dm", [(scr(8 + i), [bSCR[8 + i]] + ([bPS[2 * i], bPS[2 * i + 1]]), 128, TT) for i in range(4)])
        for m in range(8):
            evac_for_postnorm(m, m, 0)
        checkpoint("ffn_d", [(FB[:, m, 0:TT], [bFB[m]], 128, TT) for m in range(8)])
        postnorm_residual(xs, gpost, 0)

    def proj_k(T):
        for pr in range(4):
            s, bw = wget("wk", pr)
            w3 = wk3(s)
            ps = pr % 4
            for k in range(8):
                mm(PSB[ps][:, :], w3[:, k, :], scr(k), k == 0, k == 7, [bw, bSCR[k]], bPS[ps])
            vop(lambda e, pr=pr, ps=ps: e.tensor_reduce(out=KMS[:, pr, 2 * T:2 * T + 2],
                                                        in_=PSB[ps][:, :].rearrange("p (a b) -> p a b", a=2),
                                                        axis=AX.X, op=ALU.add), [bPS[ps]], [bKMS, bPS[ps]])
            aop(lambda e, pr=pr, ps=ps: e.copy(out=KT[:, pr, T * TT:(T + 1) * TT], in_=PSB[ps][:, :]), [bPS[ps]], [bKT[pr][T]])
        vop(lambda e: e.tensor_scalar(out=KMB[:, :, 2 * T:2 * T + 2], in0=KMS[:, :, 2 * T:2 * T + 2], scalar1=1.0 / 256, scalar2=None, op0=ALU.mult),
            [bKMS], [bKMB])

    def proj_v(T):
        slots = [wget("wv", pr) for pr in range(4)]
        for i in range(4):
            kt = 4 * T + i
            ps = 4 + i % 2
            for pr in range(4):
                s, bw = slots[pr]
                w3 = wk3(s)
                for k in range(8):
                    mm(PSB[ps][:, pr * 128:(pr + 1) * 128], scr(k)[:, i * 128:(i + 1) * 128], w3[:, k, :], k == 0, k == 7,
                       [bw, bSCR[k]], bPS[ps])
            aop(lambda e, kt=kt, ps=ps: e.copy(out=VA[:, kt, :, 0:64], in_=PSB[ps][:, :].rearrange("p (h d) -> p h d", h=NH)),
                [bPS[ps]], [bVA[kt]])

    def proj_q():
        for pr in range(4):
            s, bw = wget("wq", pr)
            w3 = wk3(s)
            ps = pr % 4
            for k in range(8):
                mm(PSB[ps][:, :], w3[:, k, :], scr(k), k == 0, k == 7, [bw, bSCR[k]], bPS[ps])
            for e_ in range(2):
                h = 2 * pr + e_
                lo, zlo = e_ * 64, (1 - e_) * 64
                vop(lambda e, h=h, zlo=zlo: e.memset(scr(8 + h)[zlo:zlo + 64, :], 0.0), [], [bSCR[8 + h]])
                aop(lambda e, h=h, lo=lo, ps=ps: e.mul(out=scr(8 + h)[lo:lo + 64, :], in_=PSB[ps][lo:lo + 64, :], mul=0.125),
                    [bPS[ps]], [bSCR[8 + h]])

    def proj_p_halo(hb):
        for g in range(4):
            s, bw = wget("wp", g)
            w3 = wk3(s)
            for k in range(8):
                mm(PSB[7][:, g * 16:(g + 1) * 16], w3[:, k, :], scr(k)[:, TT - 16:TT], k == 0, k == 7, [bw, bSCR[k]], bPS[7])
        vop(lambda e: e.tensor_copy(out=HALO[hb][:, :, :], in_=PSB[7][:, 0:64].rearrange("p (g t) -> p g t", g=4)),
            [bPS[7]], [bHALO[hb]])

    def moba_mask(j):
        for qi in range(4):
            for h in range(NH):
                pr, hp = h // 2, (h % 2) * 64
                mm(PSB[7][:, h * 16:(h + 1) * 16], scr(8 + h)[:, qi * 128:(qi + 1) * 128], KMB[:, pr, :],
                   True, True, [bSCR[8 + h], bKMB], bPS[7], nosig=(h < NH - 1))
            tb = 2 * j + qi // 2
            vop(lambda e, tb=tb: e.tensor_tensor(out=GT[:, :], in0=PSB[7][:, 0:128], in1=VBT[:, tb, :], op=ALU.add),
                [bPS[7], bVBT], [bGT])
            if j == 0 and qi == 0:
                checkpoint("mk_gate", [(GT[:, :], [bGT], 128, 128)])
            for h in range(NH):
                vop(lambda e, h=h: e.max(out=T8[:, h, :], in_=GT[:, h * 16:(h + 1) * 16]), [bGT], [bT8], signal=(h == NH - 1))
            vop(lambda e: e.tensor_scalar(out=THR[:, :], in0=T8[:, :, 3], scalar1=-1e30, scalar2=None, op0=ALU.max), [bT8], [bTHR])
            for h in range(NH):
                vop(lambda e, h=h: e.tensor_scalar(out=MSEL[:, h * 16:(h + 1) * 16], in0=GT[:, h * 16:(h + 1) * 16],
                                                   scalar1=THR[:, h:h + 1], scalar2=None, op0=ALU.is_lt),
                    [bGT, bTHR], [bMSEL], signal=(h == NH - 1))
            if j == 0 and qi == 0:
                checkpoint("mk_sel", [(MSEL[:, :], [bMSEL], 128, 128), (T8[:, :, :], [bT8], 128, 64)])
            for h in range(NH):
                ps = 4 + h // 4
                mm(PSB[ps][0:16, (h % 4) * 128:(h % 4 + 1) * 128], MSEL[:, h * 16:(h + 1) * 16], IDN[:, :], True, True,
                   [bMSEL, bIDN], bPS[ps], nosig=(h % 4 != 3))
            for hh in range(2):
                aop(lambda e, hh=hh, qi=qi: e.copy(out=QX[0:16, 4 * hh:4 * hh + 4, qi * 128:(qi + 1) * 128],
                                                   in_=PSB[4 + hh][0:16, :].rearrange("p (h q) -> p h q", h=4)),
                    [bPS[4 + hh]], [bQXm[4 * hh + i] for i in range(4)])

    def attention(j):
        nkt = 8 * (j + 1)
        colbase = sum(8 * (jj + 1) for jj in range(j)) * NH
        seq = [(h, kt) for h in range(NH) for kt in range(nkt)]
        LAG = 2

        def qrange(kt):
            i = kt - (nkt - 4)
            return (128 * i if i > 0 else 0), i

        def emit_s(n):
            h, kt = seq[n]
            pr, hp = h // 2, (h % 2) * 64
            q0, i = qrange(kt)
            ps = n % 4
            T, nb = kt // 4, kt // 2
            mm(PSB[ps][:, q0:TT], KT[:, pr, kt * 128:(kt + 1) * 128], scr(8 + h)[:, q0:TT], True, False,
               [bKT[pr][T], bSCR[8 + h]], bPS[ps])
            mm(PSB[ps][:, q0:TT], KX[:, nb, :], QX[:, h, q0:TT], False, True, [bKX, bQXc, bQXm[h]], bPS[ps])
            if i >= 0:
                vop(lambda e, ps=ps, q0=q0: e.tensor_tensor(out=PSB[ps][:, q0:q0 + 128], in0=PSB[ps][:, q0:q0 + 128], in1=TRI[:, :], op=ALU.add),
                    [bPS[ps], bTRI], [bPS[ps]])
            col = colbase + kt * NH + h
            pt = 24 + n % 4
            aop(lambda e, ps=ps, q0=q0, col=col, pt=pt: e.activation(out=scr(pt)[:, q0:TT], in_=PSB[ps][:, q0:TT], func=ACT.Exp,
                                                                     bias=ALB[:, col:col + 1], scale=1.0),
                [bPS[ps], bALB], [bSCR[pt]])

        def emit_pv(n):
            h, kt = seq[n]
            q0, i = qrange(kt)
            pt = 24 + n % 4
            acc = 4 + h % 2
            mm(PSB[acc][0:65, q0:TT], VA[:, kt, h, :], scr(pt)[:, q0:TT], kt == 0, kt == nkt - 1,
               [bVA[kt], bVAones, bSCR[pt]], bPS[acc], sig=True)
            if kt == nkt - 1:
                vop(lambda e, acc=acc: e.reciprocal(out=RR[64:65, :], in_=PSB[acc][64:65, :]), [bPS[acc]], [bRR])
                mm(PSB[6][0:64, :], ONES[:, 0:64], RR[:, :], True, True, [bONES, bRR], bPS[6])
                aop(lambda e: e.copy(out=BCS[:, :], in_=PSB[6][0:64, :]), [bPS[6]], [bBCS])
                vop(lambda e, acc=acc, h=h: e.tensor_tensor(out=scr(h)[0:64, :], in0=PSB[acc][0:64, :], in1=BCS[:, :], op=ALU.mult),
                    [bPS[acc], bBCS], [bSCR[h]])

        for n in range(len(seq) + LAG):
            if n < len(seq):
                emit_s(n)
            if n >= LAG:
                emit_pv(n - LAG)

    def pool_mixer(j, hb_cur, hb_prev):
        vop(lambda e: e.tensor_scalar(out=HUSE[:, :, :], in0=HALO[hb_cur][:, :, :], scalar1=HSEL[:, 2 * j:2 * j + 1], scalar2=None, op0=ALU.mult),
            [bHALO[hb_cur], bHSEL], [bHUSE])
        vop(lambda e: e.scalar_tensor_tensor(out=HUSE[:, :, :], in0=HALO[hb_prev][:, :, :], scalar=HSEL[:, 2 * j + 1:2 * j + 2],
                                             in1=HUSE[:, :, :], op0=ALU.mult, op1=ALU.add),
            [bHALO[hb_prev], bHSEL, bHUSE], [bHUSE])
        PP, TA, TB = FB[:, 0, :], FB[:, 1, :], FB[:, 2, :]
        for g in range(4):
            s, bw = wget("wp", g)
            w3 = wk3(s)
            ps = g % 4
            for k in range(8):
                mm(PSB[ps][:, :], w3[:, k, :], scr(k), k == 0, k == 7, [bw, bSCR[k]], bPS[ps])
            vop(lambda e, ps=ps: e.tensor_copy(out=PP[:, 16:528], in_=PSB[ps][:, :]), [bPS[ps]], [bFB[0]])
            vop(lambda e, g=g: e.tensor_copy(out=PP[:, 0:16], in_=HUSE[:, g, :]), [bHUSE], [bFB[0]])
            src, sb = PP, bFB[0]
            dsts = [(TA, bFB[1]), (TB, bFB[2])]
            sh = 1
            for step in range(g + 1):
                dst, db = dsts[step % 2]
                lo = 2 * sh - 1
                vop(lambda e, dst=dst, src=src, lo=lo, sh=sh: e.tensor_tensor(out=dst[:, lo:528], in0=src[:, lo:528], in1=src[:, lo - sh:528 - sh], op=ALU.add),
                    [sb], [db])
                src, sb = dst, db
                sh *= 2
            w = 2 ** (g + 1)
            vop(lambda e, g=g, src=src, w=w: e.scalar_tensor_tensor(out=scr(16 + g), in0=src[:, 16:528], scalar=1.0 / w, in1=PP[:, 16:528],
                                                                    op0=ALU.mult, op1=ALU.subtract),
                [sb, bFB[0]], [bSCR[16 + g]])
            if j == 0:
                vop(lambda e, g=g, src=src: e.tensor_tensor(out=D16[:, :], in0=src[:, 16:32], in1=INV0[:, g, :], op=ALU.mult),
                    [sb, bINV0], [bD16])
                vop(lambda e, g=g: e.tensor_tensor(out=scr(16 + g)[:, 0:16], in0=D16[:, :], in1=PP[:, 16:32], op=ALU.subtract),
                    [bD16, bFB[0]], [bSCR[16 + g]])
        for g in range(4):
            ps = g % 4
            mm(PSB[ps][:, :], PW[:, g, :], scr(16 + g), True, True, [bPW, bSCR[16 + g]], bPS[ps])
            vop(lambda e, g=g, ps=ps: e.tensor_scalar(out=scr(20 + g), in0=PSB[ps][:, :], scalar1=GA[:, G_PSC + g:G_PSC + g + 1], scalar2=None, op0=ALU.mult),
                [bPS[ps], bGA], [bSCR[20 + g]])

    def mix_out(xs):
        for m in range(8):
            sa, ba = wget("woa", m)
            sp, bp = wget("wop", m)
            wa3 = WS[0:64, sa, :].rearrange("p (h f) -> p h f", h=NH)
            wp3 = WS[:, sp, 0:512].rearrange("p (g f) -> p g f", g=4)
            ps = m % 4
            for h in range(NH):
                mm(PSB[ps][:, :], wa3[:, h, :], scr(h)[0:64, :], h == 0, False, [ba, bSCR[h]], bPS[ps])
            for g in range(4):
                mm(PSB[ps][:, :], wp3[:, g, :], scr(20 + g), False, g == 3, [bp, bSCR[20 + g]], bPS[ps])
            evac_for_postnorm(m, ps, 8)
        postnorm_residual(xs, G_MIXPOST, 8)

    def xa_setup():
        MT = FB[:, :, 0:256]
        S.dma("sync", "memld", lambda e: e.dma_start(out=MT, in_=memT), writes=bFB)
        for k in range(8):
            aop(lambda e, k=k: e.activation(out=scr(8 + k)[:, 0:256], in_=FB[:, k, 0:256], func=ACT.Square), [bFB[k]], [bSCR[8 + k]])
        for k in range(8):
            mm(PSB[6][:, 0:256], ONES[:, :], scr(8 + k)[:, 0:256], k == 0, k == 7, [bONES, bSCR[8 + k]], bPS[6])
        aop(lambda e: e.activation(out=TMP[:, 0, 0:256], in_=PSB[6][:, 0:256], func=ACT.Sqrt, bias=EPST[:, 0:1], scale=1.0 / D),
            [bPS[6], bEPS], [bTMP[0]])
        vop(lambda e: e.reciprocal(out=RT[:, 0:256], in_=TMP[:, 0, 0:256]), [bTMP[0]], [bRT])
        for k in range(8):
            vop(lambda e, k=k: e.scalar_tensor_tensor(out=scr(k)[:, 0:256], in0=FB[:, k, 0:256], scalar=GA[:, G_MEM + k:G_MEM + k + 1],
                                                      in1=RT[:, 0:256], op0=ALU.mult, op1=ALU.mult),
                [bFB[k], bGA, bRT], [bSCR[k]])
        for c in range(8):
            s, bw = wget("xk", c)
            w3 = wk3(s)
            ps = c % 4
            for k in range(8):
                mm(PSB[ps][:, 0:256], w3[:, k, :], scr(k)[:, 0:256], k == 0, k == 7, [bw, bSCR[k]], bPS[ps])
            aop(lambda e, c=c, ps=ps: e.copy(out=KMX[:, c, :], in_=PSB[ps][:, 0:256]), [bPS[ps]], [bKMX[c]])
        for c in range(8):
            s, bw = wget("xv", c)
            w3 = wk3(s)
            for mt in range(2):
                ps = 4 + mt
                for k in range(8):
                    mm(PSB[ps][:, 0:128], scr(k)[:, mt * 128:(mt + 1) * 128], w3[:, k, :], k == 0, k == 7, [bw, bSCR[k]], bPS[ps])
                aop(lambda e, c=c, mt=mt, ps=ps: e.copy(out=VMX[:, mt, c * 128:(c + 1) * 128], in_=PSB[ps][:, 0:128]), [bPS[ps]], [bVMX[mt]])

    def xattn(xs):
        prenorm(xs, G_XAPRE)
        for c in range(8):
            s, bw = wget("xq", c)
            w3 = wk3(s)
            ps = c % 4
            for k in range(8):
                mm(PSB[ps][:, :], w3[:, k, :], scr(k), k == 0, k == 7, [bw, bSCR[k]], bPS[ps])
            aop(lambda e, c=c, ps=ps: e.mul(out=scr(8 + c), in_=PSB[ps][:, :], mul=1.0 / 16), [bPS[ps]], [bSCR[8 + c]])
        for hx in range(4):
            for mt in range(2):
                ps = (2 * hx + mt) % 4
                for cc in range(2):
                    c = 2 * hx + cc
                    mm(PSB[ps][:, :], KMX[:, c, mt * 128:(mt + 1) * 128], scr(8 + c), cc == 0, cc == 1, [bKMX[c], bSCR[8 + c]], bPS[ps])
                aop(lambda e, hx=hx, mt=mt, ps=ps: e.activation(out=scr(16 + 2 * hx + mt), in_=PSB[ps][:, :], func=ACT.Exp),
                    [bPS[ps]], [bSCR[16 + 2 * hx + mt]])
            for mt in range(2):
                mm(PSB[6][:, :], ONES[:, :], scr(16 + 2 * hx + mt), mt == 0, mt == 1, [bONES, bSCR[16 + 2 * hx + mt]], bPS[6])
            vop(lambda e: e.reciprocal(out=RT[:, :], in_=PSB[6][:, :]), [bPS[6]], [bRT])
            for cc in range(2):
                c = 2 * hx + cc
                ps = 4 + cc
                for mt in range(2):
                    mm(PSB[ps][:, :], VMX[:, mt, c * 128:(c + 1) * 128], scr(16 + 2 * hx + mt), mt == 0, mt == 1,
                       [bVMX[mt], bSCR[16 + 2 * hx + mt]], bPS[ps])
                vop(lambda e, c=c, ps=ps: e.tensor_tensor(out=scr(c), in0=PSB[ps][:, :], in1=RT[:, :], op=ALU.mult),
                    [bPS[ps], bRT], [bSCR[c]])
        for m in range(8):
            s, bw = wget("xo", m)
            w3 = wk3(s)
            ps = m % 4
            for k in range(8):
                mm(PSB[ps][:, :], w3[:, k, :], scr(k), k == 0, k == 7, [bw, bSCR[k]], bPS[ps])
            evac_for_postnorm(m, ps, 8)
        postnorm_residual(xs, G_XAPOST, 8)

    def load_x(T, xs):
        for k in range(8):
            S.dma("sync", "x%d_%d" % (xs, k), lambda e, k=k: e.dma_start(out=XS[xs][:, k, :], in_=xT[T, :, k, :]), writes=[bXS[xs][k]])

    s_pw, b_pw = None, None
    S.dma("sync", "pwld", lambda e: e.dma_start(out=PW[:, :, :], in_=w_sc_d["pw"][0].rearrange("p (g f) -> p g f", g=4)),
          reads=[w_sc_buf["pw"][0]], writes=[bPW])
    out_toks = []

    def store_out(j):
        for k in range(8):
            out_toks.append(S.dma("sync", "o%d" % k, lambda e, k=k, j=j: e.dma_start(out=outT[j, :, k, :], in_=XS[1][:, k, :]),
                                  reads=[bXS[1][k]]))

    def xs_items(xs):
        return [(XS[xs][:, k, :], [bXS[xs][k]], 128, TT) for k in range(8)]

    def scr_items(idx, P=128):
        return [(scr(i)[0:P, :], [bSCR[i]], P, TT) for i in idx]

    def main_body():
        load_x(0, 0)
        load_x(1, 1)
        checkpoint("xload", xs_items(0))
        for j in range(4):
            T_other, T_own = 2 * j, 2 * j + 1

            def hook(j=j, T_own=T_own):
                if j > 0:
                    store_out(j - 1)
                    load_x(T_own, 1)

            if j == 0 and stage == "prenorm":
                prenorm(0, G_F1PRE)
                checkpoint("prenorm", scr_items(range(8)))
            ffn(0, "g1", "u1", "d1", G_F1PRE, G_F1POST, mid_hook=hook)
            if j == 0:
                checkpoint("ffn1", xs_items(0))
            prenorm(0, G_MIXPRE)
            proj_k(T_other)
            proj_v(T_other)
            proj_p_halo(j % 2)
            if j == 0:
                checkpoint("kv0", [(KT[:, pr, 0:TT], [bKT[pr][0]], 128, TT) for pr in range(4)]
                           + [(VA[:, i, :, 0:64], [bVA[i]], 128, TT) for i in range(4)])
            ffn(1, "g1", "u1", "d1", G_F1PRE, G_F1POST)
            if j < 3:
                load_x(T_other + 2, 0)
            prenorm(1, G_MIXPRE)
            proj_k(T_own)
            proj_q()
            if j == 0:
                checkpoint("q0", scr_items(range(8, 16)))
            moba_mask(j)
            proj_v(T_own)
            if j == 0:
                checkpoint("mask0", [(QX[0:18, h, :], [bQXm[h], bQXc], 18, TT) for h in range(8)])
            pool_mixer(j, j % 2, (j + 1) % 2)
            if j == 0:
                checkpoint("pool0", scr_items(range(20, 24)))
            attention(j)
            if j == 0:
                checkpoint("attn0", scr_items(range(8), 64))
            if j == 1:
                checkpoint("attn1", scr_items(range(8), 64))
            mix_out(1)
            if j == 0:
                checkpoint("mix0", xs_items(1))
                xa_setup()
            xattn(1)
            if j == 0:
                checkpoint("xa0", xs_items(1))
            ffn(1, "g2", "u2", "d2", G_F2PRE, G_F2POST)
        store_out(3)

    try:
        main_body()
    except _Stop:
        pass
    S.final_wait("sync", out_toks)
    S.final_wait("gpsimd", dbg_toks)
    S.emit()
    return nc


def _chunked(w):
    n = w.shape[1]
    return np.ascontiguousarray(w.reshape(8, 128, n // 128, 128).transpose(2, 1, 0, 3)).reshape(n // 128, 128, 1024)


def _gcols(v):
    return np.ascontiguousarray(v.reshape(-1, 128).T)


def _const_tables(p):
    slopes = (2.0 ** (-8.0 * np.arange(1, NH + 1) / NH)).astype(np.float64)
    true_tile = lambda T: 2 * (T // 2) + ((1 - p) if T % 2 == 0 else p)
    kj = np.arange(128)
    alb = np.zeros((128, 640), np.float64)
    col = 0
    for j in range(4):
        qbase = TT * (2 * j + p)
        for kt in range(8 * (j + 1)):
            kbase = TT * true_tile(kt // 4) + 128 * (kt % 4)
            for h in range(NH):
                if kbase >= qbase + TT:
                    alb[:, col] = NEG
                else:
                    alb[:, col] = slopes[h] * (kbase + kj - qbase)
                col += 1
    vb = np.zeros((8, 16), np.float64)
    for j in range(4):
        for half in range(2):
            own_true_blk = 2 * (2 * j + p) + half
            for nb in range(16):
                tb = 2 * true_tile(nb // 2) + nb % 2
                if nb // 2 > 2 * j + 1:
                    v = -2e30
                elif tb == own_true_blk:
                    v = 1e30
                elif tb < own_true_blk:
                    v = 0.0
                else:
                    v = -2e30
                vb[2 * j + half, nb] = v
    vbt = np.broadcast_to(np.tile(vb[:, None, :], (1, NH, 1)).reshape(1, 8 * 128), (128, 8 * 128))
    tri = np.where(kj[:, None] <= kj[None, :], 0.0, NEG)
    identn = NEG * np.eye(128)
    q = np.arange(TT)
    qxc = np.zeros((2, NH, TT), np.float64)
    for h in range(NH):
        qxc[0, h] = -slopes[h] * 256 * (q // 256)
        qxc[1, h] = -slopes[h] * (q % 256)
    kx = np.zeros((18, 16, 128), np.float64)
    for nb in range(16):
        kx[nb, nb, :] = 1.0
    kx[16:18] = 1.0
    inv0 = np.zeros((4, 16), np.float64)
    for g in range(4):
        w = 2 ** (g + 1)
        for t in range(16):
            inv0[g, t] = 1.0 / (min(t + 1, w) if p == 0 else w)
    hsel = np.zeros(8, np.float64)
    for j in range(4):
        if p == 1:
            hsel[2 * j] = 1.0
        elif j > 0:
            hsel[2 * j + 1] = 1.0
    f = lambda a: np.ascontiguousarray(a, dtype=np.float32)
    return {
        "albias": f(alb), "vb": f(vbt), "tri": f(tri), "identn": f(identn), "qxc": f(qxc.reshape(2, NH * TT)),
        "kx": f(kx.reshape(18, 16 * 128)), "inv0": f(np.broadcast_to(inv0.reshape(1, 64), (128, 64))),
        "hsel": f(np.broadcast_to(hsel[None, :], (128, 8))),
    }


_NC_CACHE = {}
_DEV = {}


def kernel(x, mem, ffn1_pre_g, ffn1_w_gate, ffn1_w_up, ffn1_w_down, ffn1_post_g,
           mix_pre_g, w_in, pool_w, pool_scale, w_out, mix_post_g,
           xa_pre_g, mem_g, xa_wq, xa_wkv, xa_wo, xa_post_g,
           ffn2_pre_g, ffn2_w_gate, ffn2_w_up, ffn2_w_down, ffn2_post_g):
    a = lambda t: np.asarray(t, dtype=np.float32)
    x, mem = a(x), a(mem)
    w_in0, w_out0, xa_wkv0 = a(w_in)[0], a(w_out)[0], a(xa_wkv)[0]
    W = {
        "g1": _chunked(a(ffn1_w_gate)[0]), "u1": _chunked(a(ffn1_w_up)[0]), "d1": np.ascontiguousarray(a(ffn1_w_down)[0].reshape(NFC, 128, 1024)),
        "g2": _chunked(a(ffn2_w_gate)[0]), "u2": _chunked(a(ffn2_w_up)[0]), "d2": np.ascontiguousarray(a(ffn2_w_down)[0].reshape(NFC, 128, 1024)),
        "wq": _chunked(w_in0[:, 0:512]), "wk": _chunked(w_in0[:, 512:1024]), "wv": _chunked(w_in0[:, 1024:1536]),
        "wp": _chunked(w_in0[:, 1536:2048]),
        "woa": np.ascontiguousarray(w_out0[:512].reshape(8, 64, 8, 128).transpose(2, 1, 0, 3)).reshape(8, 64, 1024),
        "wop": np.ascontiguousarray(w_out0[512:].reshape(4, 128, 8, 128).transpose(2, 1, 0, 3)).reshape(8, 128, 512),
        "xq": _chunked(a(xa_wq)[0]), "xk": _chunked(xa_wkv0[:, :1024]), "xv": _chunked(xa_wkv0[:, 1024:]),
        "xo": _chunked(a(xa_wo)[0]),
        "pw": np.ascontiguousarray(a(pool_w)[0].transpose(1, 0, 2)).reshape(1, 128, 512),
    }
    gains = np.concatenate([_gcols(a(g)[0]) for g in (ffn1_pre_g, ffn1_post_g, mix_pre_g, mix_post_g, xa_pre_g, mem_g,
                                                       xa_post_g, ffn2_pre_g, ffn2_post_g, pool_scale)], axis=1)
    gains = np.ascontiguousarray(gains, dtype=np.float32)
    assert gains.shape == (128, NGCOL)
    tabs = [_const_tables(0), _const_tables(1)]
    in_maps = []
    for c in range(8):
        b, p = c // 2, c % 2
        order = []
        for j in range(4):
            order += [2 * j + 1 - p, 2 * j + p]
        xb = x[b].reshape(8, TT, 8, 128)
        xTc = np.ascontiguousarray(xb[order].transpose(0, 3, 2, 1))
        memTc = np.ascontiguousarray(mem[b].reshape(256, 8, 128).transpose(2, 1, 0))
        m = {"xT": xTc, "memT": memTc, "gains": gains}
        m.update(tabs[p])
        for fam, arr in W.items():
            m["w_" + fam] = arr
        in_maps.append(m)
    if _DEV.get("stage"):
        return in_maps
    if "nc" not in _NC_CACHE:
        _NC_CACHE["nc"] = build_program()
    res = run_bass_kernel_spmd(_NC_CACHE["nc"], in_maps, core_ids=list(range(8)))
    out = np.empty((NBATCH, SEQ, D), np.float32)
    for c in range(8):
        b, p = c // 2, c % 2
        o = np.asarray(res.results[c]["outT"])
        for j in range(4):
            t = 2 * j + p
            out[b, t * TT:(t + 1) * TT, :] = o[j].transpose(2, 1, 0).reshape(TT, D)
    return out
```

```python
from contextlib import ExitStack
import numpy as np
import concourse.bass as bass
import concourse.mybir as mybir
from concourse.bass_utils import run_bass_kernel_spmd

F32 = mybir.dt.float32
BF16 = mybir.dt.bfloat16
ALU = mybir.AluOpType
ACT = mybir.ActivationFunctionType
AX = mybir.AxisListType

D = 1024
SEQ = 4096
NBATCH = 4
DFF = 2816
NFC = DFF // 128
TT = 512
NH = 8
NEG = -30000.0
EPS = 1e-6
NSLOT = 8


class Buf:
    __slots__ = ("name", "last_write", "reads")

    def __init__(self, name):
        self.name = name
        self.last_write = None
        self.reads = []


class EngState:
    def __init__(self, name):
        self.name = name
        self.count = 0
        self.items = []
        self.seen = {}


class Sched:
    ENG = ("tensor", "vector", "scalar", "gpsimd", "sync")

    def __init__(self, nc):
        self.nc = nc
        self.es = ExitStack()
        self.eng = {n: EngState(n) for n in self.ENG}
        self.sems = {}
        for n in self.ENG:
            self.sems[("eng", n)] = self.es.enter_context(nc.semaphore("sem_" + n))
        self.chan_count = {}

    def chan(self, name):
        key = ("chan", name)
        if key not in self.sems:
            self.sems[key] = self.es.enter_context(self.nc.semaphore("ch_" + name))
            self.chan_count[key] = 0
        return key

    def _wait(self, e, tok):
        key, val = tok
        if key == ("eng", e.name) and val > e.count:
            return
        if e.seen.get(key, 0) >= val:
            return
        e.seen[key] = val
        e.items.append(("wait", key, val))

    def _deps(self, e, reads, writes):
        own = ("eng", e.name)
        skip_own = e.name == "tensor"
        for b in reads:
            t = b.last_write
            if t is not None and not (skip_own and t[0] == own):
                self._wait(e, t)
        for b in writes:
            t = b.last_write
            if t is not None and not (skip_own and t[0] == own):
                self._wait(e, t)
            for r in b.reads:
                if not (skip_own and r[0] == own):
                    self._wait(e, r)

    @staticmethod
    def _mark(tok, reads, writes):
        for b in writes:
            b.last_write = tok
            b.reads = []
        for b in reads:
            rl = b.reads
            if rl and rl[-1][0] == tok[0]:
                rl[-1] = tok
            else:
                rl.append(tok)

    def op(self, engname, fn, reads=(), writes=(), signal=True):
        e = self.eng[engname]
        self._deps(e, reads, writes)
        tok = (("eng", engname), e.count + 1)
        if signal:
            e.count += 1
        e.items.append(("op", fn, signal))
        self._mark(tok, reads, writes)
        return tok

    def dma(self, queue, chan, fn, reads=(), writes=()):
        e = self.eng[queue]
        key = self.chan(chan)
        if self.chan_count[key] > 0:
            self._wait(e, (key, self.chan_count[key]))
        self._deps(e, reads, writes)
        self.chan_count[key] += 16
        tok = (key, self.chan_count[key])
        e.items.append(("dma", fn, key))
        self._mark(tok, reads, writes)
        return tok

    def final_wait(self, engname, toks):
        e = self.eng[engname]
        for t in toks:
            self._wait(e, t)

    def emit(self):
        nc = self.nc
        sems = self.sems

        def run(e, engobj):
            own = sems[("eng", e.name)]
            for it in e.items:
                if it[0] == "wait":
                    engobj.wait_ge(sems[it[1]], it[2])
                elif it[0] == "op":
                    ins = it[1](engobj)
                    if it[2]:
                        ins.then_inc(own, 1)
                else:
                    it[1](engobj).then_inc(sems[it[2]], 16)

        with nc.allow_low_precision("bf16 matmul operands, fp32 accumulation by design"), nc.Block() as block:
            @block.tensor
            def _(t):
                run(self.eng["tensor"], t)

            @block.vector
            def _(v):
                run(self.eng["vector"], v)

            @block.scalar
            def _(s):
                run(self.eng["scalar"], s)

            @block.gpsimd
            def _(g):
                run(self.eng["gpsimd"], g)

            @block.sync
            def _(sy):
                run(self.eng["sync"], sy)
        self.es.close()


WFAMS = {
    "g1": (NFC, 128, 1024), "u1": (NFC, 128, 1024), "d1": (NFC, 128, 1024),
    "g2": (NFC, 128, 1024), "u2": (NFC, 128, 1024), "d2": (NFC, 128, 1024),
    "wq": (4, 128, 1024), "wk": (4, 128, 1024), "wv": (4, 128, 1024), "wp": (4, 128, 1024),
    "woa": (8, 64, 1024), "wop": (8, 128, 512),
    "xq": (8, 128, 1024), "xk": (8, 128, 1024), "xv": (8, 128, 1024), "xo": (8, 128, 1024),
    "pw": (1, 128, 512),
}
CONV_ORDER = ["g1", "u1", "d1", "wk", "wv", "wq", "wp", "pw", "woa", "wop", "xk", "xv", "xq", "xo",
              "g2", "u2", "d2"]
G_F1PRE, G_F1POST, G_MIXPRE, G_MIXPOST, G_XAPRE, G_MEM, G_XAPOST, G_F2PRE, G_F2POST, G_PSC = [8 * i for i in range(10)]
NGCOL = 76


class _Stop(Exception):
    pass


def build_program(stage=None):
    nc = bass.Bass("TRN2", target_bir_lowering=False)
    S = Sched(nc)
    dbg = nc.dram_tensor("dbg", [128, 8 * TT], F32, kind="ExternalOutput").ap() if stage else None
    dbg_toks = []

    def checkpoint(name, items):
        if stage != name:
            return
        off = 0
        for i, (ap, bufs, P, n) in enumerate(items):
            dbg_toks.append(S.dma("gpsimd", "dbg%d" % (i % 4), lambda e, ap=ap, off=off, P=P, n=n: e.dma_start(out=dbg[0:P, off:off + n], in_=ap), reads=bufs))
            off += n
        raise _Stop()

    def din(name, shape, dt=F32):
        return nc.dram_tensor(name, list(shape), dt, kind="ExternalInput").ap()

    xT = din("xT", [8, 128, 8, TT])
    memT = din("memT", [128, 8, 256])
    gains_d = din("gains", [128, NGCOL])
    albias_d = din("albias", [128, 640])
    vb_d = din("vb", [128, 8 * 128])
    tri_d = din("tri", [128, 128])
    identn_d = din("identn", [128, 128])
    qxc_d = din("qxc", [2, NH * TT])
    kx_d = din("kx", [18, 16 * 128])
    inv0_d = din("inv0", [128, 64])
    hsel_d = din("hsel", [128, 8])
    outT = nc.dram_tensor("outT", [4, 128, 8, TT], F32, kind="ExternalOutput").ap()
    w_in_d, w_sc_d, w_sc_buf = {}, {}, {}
    for fam, (n, P, X) in WFAMS.items():
        w_in_d[fam] = din("w_" + fam, [n, P, X])
        w_sc_d[fam] = nc.dram_tensor("sc_" + fam, [n, P, X], BF16, kind="Internal").ap()
        w_sc_buf[fam] = [Buf("sc_%s_%d" % (fam, c)) for c in range(n)]

    A = nc.alloc_sbuf_tensor
    KT = A("KT", [128, 4, SEQ], BF16)
    VA = A("VA", [128, 32, NH, 65], BF16)
    KX = A("KX", [18, 16, 128], BF16)
    QX = A("QX", [18, NH, TT], BF16)
    XS = [A("XS%d" % i, [128, 8, TT], F32) for i in range(2)]
    FB = A("FB", [128, 8, 528], F32)
    SCR = A("SCR", [128, 30, TT], BF16)
    WS = A("WS", [128, NSLOT, 1024], BF16)
    KMX = A("KMX", [128, 8, 256], BF16)
    VMX = A("VMX", [128, 2, 1024], BF16)
    RT = A("RT", [128, TT], F32)
    TMP = A("TMP", [128, 2, TT], F32)
    GA = A("GA", [128, NGCOL], F32)
    ALB = A("ALB", [128, 640], F32)
    VBT = A("VBT", [128, 8, 128], F32)
    TRI = A("TRI", [128, 128], F32)
    IDN = A("IDN", [128, 128], BF16)
    ONES = A("ONES", [128, 128], BF16)
    EPST = A("EPST", [128, 1], F32)
    PW = A("PW", [128, 4, 128], BF16)
    KMS = A("KMS", [128, 4, 16], F32)
    KMB = A("KMB", [128, 4, 16], BF16)
    GT = A("GT", [128, 128], F32)
    T8 = A("T8", [128, NH, 8], F32)
    THR = A("THR", [128, NH], F32)
    MSEL = A("MSEL", [128, 128], BF16)
    RR = A("RR", [128, TT], BF16)
    BCS = A("BCS", [64, TT], F32)
    HALO = [A("HALO%d" % i, [128, 4, 16], F32) for i in range(2)]
    HUSE = A("HUSE", [128, 4, 16], F32)
    INV0 = A("INV0", [128, 4, 16], F32)
    HSEL = A("HSEL", [128, 8], F32)
    D16 = A("D16", [128, 16], F32)
    PSB = [nc.alloc_psum_tensor("PS%d" % i, [128, TT], F32) for i in range(8)]

    bKT = [[Buf("KT%d_%d" % (pr, T)) for T in range(8)] for pr in range(4)]
    bVA = [Buf("VA%d" % kt) for kt in range(32)]
    bVAones = Buf("VAones")
    bKX, bQXc = Buf("KX"), Buf("QXc")
    bQXm = [Buf("QXm%d" % h) for h in range(NH)]
    bXS = [[Buf("XS%d_%d" % (i, k)) for k in range(8)] for i in range(2)]
    bFB = [Buf("FB%d" % k) for k in range(8)]
    bSCR = [Buf("SCR%d" % i) for i in range(30)]
    bWS = [Buf("WS%d" % i) for i in range(NSLOT)]
    bKMX, bVMX = [Buf("KMX%d" % i) for i in range(8)], [Buf("VMX%d" % i) for i in range(2)]
    bRT, bTMP = Buf("RT"), [Buf("TMP0"), Buf("TMP1")]
    bGA, bALB, bVBT, bTRI, bIDN, bONES, bEPS, bPW = (Buf(n) for n in "GA ALB VBT TRI IDN ONES EPS PW".split())
    bKMS, bKMB, bGT, bT8, bTHR, bMSEL, bRR, bBCS = (Buf(n) for n in "KMS KMB GT T8 THR MSEL RR BCS".split())
    bHALO, bHUSE, bINV0, bHSEL, bD16 = [Buf("HALO0"), Buf("HALO1")], Buf("HUSE"), Buf("INV0"), Buf("HSEL"), Buf("D16")
    bPS = [Buf("PS%d" % i) for i in range(8)]

    def scr(i):
        return SCR[:, i, :]

    def vop(fn, reads, writes, signal=True):
        S.op("vector", fn, reads, writes, signal=signal)

    def aop(fn, reads, writes):
        S.op("scalar", fn, reads, writes)

    def mm(out, lhsT, rhs, start, stop, reads, wbuf, sig=False, nosig=False):
        S.op("tensor", lambda e, out=out, lhsT=lhsT, rhs=rhs, start=start, stop=stop:
             e.matmul(out, lhsT=lhsT, rhs=rhs, start=start, stop=stop),
             reads, [wbuf], signal=((stop or sig) and not nosig))

    cload = [0]

    def ld_const(dst, src, buf, queue="sync"):
        cload[0] += 1
        S.dma(queue, ("c%d" if queue == "sync" else "cg%d") % (cload[0] % 4), lambda e, dst=dst, src=src: e.dma_start(out=dst, in_=src), writes=[buf])

    S.op("gpsimd", lambda e: e.memset(ONES[:, :], 1.0), writes=[bONES])
    S.op("gpsimd", lambda e: e.memset(EPST[:, :], EPS), writes=[bEPS])
    S.op("gpsimd", lambda e: e.memset(RR[:, :], 0.0), writes=[bRR])
    S.op("gpsimd", lambda e: e.memset(KMB[:, :, :], 0.0), writes=[bKMB])
    S.op("gpsimd", lambda e: e.memset(VA[:, :, :, 64:65], 1.0), writes=[bVAones])
    S.op("gpsimd", lambda e: e.memset(HALO[0][:, :, :], 0.0), writes=[bHALO[0]])
    S.op("gpsimd", lambda e: e.memset(HALO[1][:, :, :], 0.0), writes=[bHALO[1]])
    ld_const(GA[:, :], gains_d, bGA)
    ld_const(ALB[:, :], albias_d, bALB)
    ld_const(VBT[:, :, :], vb_d.rearrange("p (a b) -> p a b", a=8), bVBT)
    ld_const(TRI[:, :], tri_d, bTRI)
    ld_const(INV0[:, :, :], inv0_d.rearrange("p (a b) -> p a b", a=4), bINV0)
    ld_const(HSEL[:, :], hsel_d, bHSEL)
    ld_const(IDN[:, :], identn_d, bIDN, "gpsimd")
    ld_const(QX[16:18, :, :], qxc_d.rearrange("p (a b) -> p a b", a=NH), bQXc, "gpsimd")
    ld_const(KX[:, :, :], kx_d.rearrange("p (a b) -> p a b", a=16), bKX, "gpsimd")
    vop(lambda e: e.tensor_scalar(out=GA[:, G_F1POST:G_F1POST + 8], in0=GA[:, G_F1POST:G_F1POST + 8], scalar1=0.5, scalar2=None, op0=ALU.mult), [bGA], [bGA])
    vop(lambda e: e.tensor_scalar(out=GA[:, G_F2POST:G_F2POST + 8], in0=GA[:, G_F2POST:G_F2POST + 8], scalar1=0.5, scalar2=None, op0=ALU.mult), [bGA], [bGA])

    cv = [0]
    import os as _os
    CVG = int(_os.environ.get("CVG", "8"))
    for fam in CONV_ORDER:
        n = WFAMS[fam][0]
        for c0 in range(0, n, CVG):
            c1 = min(n, c0 + CVG)
            cv[0] += 1
            S.dma("gpsimd", "cv%d" % (cv[0] % 8),
                  lambda e, fam=fam, c0=c0, c1=c1: e.dma_start(out=w_sc_d[fam][c0:c1], in_=w_in_d[fam][c0:c1]),
                  writes=[w_sc_buf[fam][c] for c in range(c0, c1)])

    wring = [0]

    def wget(fam, c):
        n, P, X = WFAMS[fam]
        s = wring[0] % NSLOT
        wring[0] += 1
        S.dma("sync", "ws%d" % s,
              lambda e, fam=fam, c=c, s=s, P=P, X=X: e.dma_start(out=WS[0:P, s, 0:X], in_=w_sc_d[fam][c]),
              reads=[w_sc_buf[fam][c]], writes=[bWS[s]])
        return s, bWS[s]

    def wk3(s):
        return WS[:, s, :].rearrange("p (k f) -> p k f", k=8)

    def norm_stats(sq_tiles):
        for k, t in enumerate(sq_tiles):
            mm(PSB[6][:, :], ONES[:, :], scr(t), k == 0, k == 7, [bONES, bSCR[t]], bPS[6])
        aop(lambda e: e.activation(out=TMP[:, 0, :], in_=PSB[6][:, :], func=ACT.Sqrt, bias=EPST[:, 0:1], scale=1.0 / D),
            [bPS[6], bEPS], [bTMP[0]])
        vop(lambda e: e.reciprocal(out=RT[:, :], in_=TMP[:, 0, :]), [bTMP[0]], [bRT])

    def prenorm(xs, gcol, ntok=TT, sq_base=8):
        X = XS[xs]
        for k in range(8):
            aop(lambda e, k=k: e.activation(out=scr(sq_base + k), in_=X[:, k, :], func=ACT.Square),
                [bXS[xs][k]], [bSCR[sq_base + k]])
        norm_stats([sq_base + k for k in range(8)])
        for k in range(8):
            vop(lambda e, k=k: e.scalar_tensor_tensor(out=scr(k), in0=X[:, k, :], scalar=GA[:, gcol + k:gcol + k + 1],
                                                      in1=RT[:, :], op0=ALU.mult, op1=ALU.mult),
                [bXS[xs][k], bGA, bRT], [bSCR[k]])

    def evac_for_postnorm(m, ps_i, sq_base):
        vop(lambda e, m=m, ps_i=ps_i: e.tensor_copy(out=FB[:, m, 0:TT], in_=PSB[ps_i][:, :]), [bPS[ps_i]], [bFB[m]])
        aop(lambda e, m=m: e.activation(out=scr(sq_base + m), in_=FB[:, m, 0:TT], func=ACT.Square),
            [bFB[m]], [bSCR[sq_base + m]])

    def postnorm_residual(xs, gcol, sq_base):
        X = XS[xs]
        norm_stats([sq_base + m for m in range(8)])
        for m in range(8):
            vop(lambda e, m=m: e.scalar_tensor_tensor(out=FB[:, m, 0:TT], in0=FB[:, m, 0:TT], scalar=GA[:, gcol + m:gcol + m + 1],
                                                      in1=RT[:, :], op0=ALU.mult, op1=ALU.mult),
                [bFB[m], bGA, bRT], [bFB[m]])
            vop(lambda e, m=m: e.tensor_tensor(out=X[:, m, :], in0=X[:, m, :], in1=FB[:, m, 0:TT], op=ALU.add),
                [bXS[xs][m], bFB[m]], [bXS[xs][m]])

    def ffn(xs, fg, fu, fd, gpre, gpost, mid_hook=None):
        prenorm(xs, gpre)
        for c in range(NFC):
            if c == 6 and mid_hook is not None:
                mid_hook()
            sg, bg = wget(fg, c)
            su, bu = wget(fu, c)
            pg, pu = c % 2, 2 + c % 2
            wg3, wu3 = wk3(sg), wk3(su)
            for k in range(8):
                mm(PSB[pg][:, :], wg3[:, k, :], scr(k), k == 0, k == 7, [bg, bSCR[k]], bPS[pg])
            for k in range(8):
                mm(PSB[pu][:, :], wu3[:, k, :], scr(k), k == 0, k == 7, [bu, bSCR[k]], bPS[pu])
            aop(lambda e, c=c, pg=pg: e.activation(out=TMP[:, c % 2, :], in_=PSB[pg][:, :], func=ACT.Silu),
                [bPS[pg]], [bTMP[c % 2]])
            vop(lambda e, c=c, pu=pu: e.tensor_tensor(out=scr(8 + c), in0=PSB[pu][:, :], in1=TMP[:, c % 2, :], op=ALU.mult),
                [bPS[pu], bTMP[c % 2]], [bSCR[8 + c]])
            if c == 0:
                checkpoint("ffn_a0", [(scr(8), [bSCR[8]], 128, TT)])
            if c == 3:
                checkpoint("ffn_a3", [(scr(8 + i), [bSCR[8 + i]], 128, TT) for i in range(4)])
            checkpoint("ffn_c%d" % c, [(scr(8 + i), [bSCR[8 + i]], 128, TT) for i in range(max(0, c - 3), c + 1)])
        checkpoint("ffn_a", [(scr(8 + i), [bSCR[8 + i]], 128, TT) for i in range(14, 22)])
        import os
        MLIM = int(os.environ.get("MLIM", "8"))
        for c in range(NFC):
            sd, bd = wget(fd, c)
            for m in range(MLIM):
                mm(PSB[m][:, :], WS[:, sd, m * 128:(m + 1) * 128], scr(8 + c), c == 0, c == NFC - 1,
                   [bd, bSCR[8 + c]], bPS[m], sig=(m == MLIM - 1))
        checkpoint("ffn_dm", [(scr(8 + i), [bSCR[8 + i]] + ([bPS[2 * i], bPS[2 * i + 1]]), 128, TT) for i in range(4)])
        for m in range(8):
            evac_for_postnorm(m, m, 0)
        checkpoint("ffn_d", [(FB[:, m, 0:TT], [bFB[m]], 128, TT) for m in range(8)])
        postnorm_residual(xs, gpost, 0)

    def proj_k(T):
        for pr in range(4):
            s, bw = wget("wk", pr)
            w3 = wk3(s)
            ps = pr % 4
            for k in range(8):
                mm(PSB[ps][:, :], w3[:, k, :], scr(k), k == 0, k == 7, [bw, bSCR[k]], bPS[ps])
            vop(lambda e, pr=pr, ps=ps: e.tensor_reduce(out=KMS[:, pr, 2 * T:2 * T + 2],
                                                        in_=PSB[ps][:, :].rearrange("p (a b) -> p a b", a=2),
                                                        axis=AX.X, op=ALU.add), [bPS[ps]], [bKMS, bPS[ps]])
            aop(lambda e, pr=pr, ps=ps: e.copy(out=KT[:, pr, T * TT:(T + 1) * TT], in_=PSB[ps][:, :]), [bPS[ps]], [bKT[pr][T]])
        vop(lambda e: e.tensor_scalar(out=KMB[:, :, 2 * T:2 * T + 2], in0=KMS[:, :, 2 * T:2 * T + 2], scalar1=1.0 / 256, scalar2=None, op0=ALU.mult),
            [bKMS], [bKMB])

    def proj_v(T):
        slots = [wget("wv", pr) for pr in range(4)]
        for i in range(4):
            kt = 4 * T + i
            ps = 4 + i % 2
            for pr in range(4):
                s, bw = slots[pr]
                w3 = wk3(s)
                for k in range(8):
                    mm(PSB[ps][:, pr * 128:(pr + 1) * 128], scr(k)[:, i * 128:(i + 1) * 128], w3[:, k, :], k == 0, k == 7,
                       [bw, bSCR[k]], bPS[ps])
            aop(lambda e, kt=kt, ps=ps: e.copy(out=VA[:, kt, :, 0:64], in_=PSB[ps][:, :].rearrange("p (h d) -> p h d", h=NH)),
                [bPS[ps]], [bVA[kt]])

    def proj_q():
        for pr in range(4):
            s, bw = wget("wq", pr)
            w3 = wk3(s)
            ps = pr % 4
            for k in range(8):
                mm(PSB[ps][:, :], w3[:, k, :], scr(k), k == 0, k == 7, [bw, bSCR[k]], bPS[ps])
            for e_ in range(2):
                h = 2 * pr + e_
                lo, zlo = e_ * 64, (1 - e_) * 64
                vop(lambda e, h=h, zlo=zlo: e.memset(scr(8 + h)[zlo:zlo + 64, :], 0.0), [], [bSCR[8 + h]])
                aop(lambda e, h=h, lo=lo, ps=ps: e.mul(out=scr(8 + h)[lo:lo + 64, :], in_=PSB[ps][lo:lo + 64, :], mul=0.125),
                    [bPS[ps]], [bSCR[8 + h]])

    def proj_p_halo(hb):
        for g in range(4):
            s, bw = wget("wp", g)
            w3 = wk3(s)
            for k in range(8):
                mm(PSB[7][:, g * 16:(g + 1) * 16], w3[:, k, :], scr(k)[:, TT - 16:TT], k == 0, k == 7, [bw, bSCR[k]], bPS[7])
        vop(lambda e: e.tensor_copy(out=HALO[hb][:, :, :], in_=PSB[7][:, 0:64].rearrange("p (g t) -> p g t", g=4)),
            [bPS[7]], [bHALO[hb]])

    def moba_mask(j):
        for qi in range(4):
            for h in range(NH):
                pr, hp = h // 2, (h % 2) * 64
                mm(PSB[7][:, h * 16:(h + 1) * 16], scr(8 + h)[:, qi * 128:(qi + 1) * 128], KMB[:, pr, :],
                   True, True, [bSCR[8 + h], bKMB], bPS[7], nosig=(h < NH - 1))
            tb = 2 * j + qi // 2
            vop(lambda e, tb=tb: e.tensor_tensor(out=GT[:, :], in0=PSB[7][:, 0:128], in1=VBT[:, tb, :], op=ALU.add),
                [bPS[7], bVBT], [bGT])
            if j == 0 and qi == 0:
                checkpoint("mk_gate", [(GT[:, :], [bGT], 128, 128)])
            for h in range(NH):
                vop(lambda e, h=h: e.max(out=T8[:, h, :], in_=GT[:, h * 16:(h + 1) * 16]), [bGT], [bT8], signal=(h == NH - 1))
            vop(lambda e: e.tensor_scalar(out=THR[:, :], in0=T8[:, :, 3], scalar1=-1e30, scalar2=None, op0=ALU.max), [bT8], [bTHR])
            for h in range(NH):
                vop(lambda e, h=h: e.tensor_scalar(out=MSEL[:, h * 16:(h + 1) * 16], in0=GT[:, h * 16:(h + 1) * 16],
                                                   scalar1=THR[:, h:h + 1], scalar2=None, op0=ALU.is_lt),
                    [bGT, bTHR], [bMSEL], signal=(h == NH - 1))
            if j == 0 and qi == 0:
                checkpoint("mk_sel", [(MSEL[:, :], [bMSEL], 128, 128), (T8[:, :, :], [bT8], 128, 64)])
            for h in range(NH):
                ps = 4 + h // 4
                mm(PSB[ps][0:16, (h % 4) * 128:(h % 4 + 1) * 128], MSEL[:, h * 16:(h + 1) * 16], IDN[:, :], True, True,
                   [bMSEL, bIDN], bPS[ps], nosig=(h % 4 != 3))
            for hh in range(2):
                aop(lambda e, hh=hh, qi=qi: e.copy(out=QX[0:16, 4 * hh:4 * hh + 4, qi * 128:(qi + 1) * 128],
                                                   in_=PSB[4 + hh][0:16, :].rearrange("p (h q) -> p h q", h=4)),
                    [bPS[4 + hh]], [bQXm[4 * hh + i] for i in range(4)])

    def attention(j):
        nkt = 8 * (j + 1)
        colbase = sum(8 * (jj + 1) for jj in range(j)) * NH
        slopes_ = [2.0 ** (-(h + 1)) for h in range(NH)]

        def tile_true(T, p):
            return 2 * (T // 2) + ((1 - p) if T % 2 == 0 else p)

        def skippable(h, kt):
            T = kt // 4
            if T == 2 * j + 1:
                return False
            for p in (0, 1):
                qbase = TT * (2 * j + p)
                kbase = TT * tile_true(T, p) + 128 * (kt % 4)
                if kbase >= qbase + TT:
                    continue
                if slopes_[h] * (qbase - (kbase + 127)) <= 80.0:
                    return False
            return True

        seq = [(h, kt) for h in range(NH) for kt in range(nkt) if not skippable(h, kt)]
        first_kt = {h: min(kt for (hh, kt) in seq if hh == h) for h in range(NH)}
        LAG = 2

        def qrange(kt):
            i = kt - (nkt - 4)
            return (128 * i if i > 0 else 0), i

        def emit_s(n):
            h, kt = seq[n]
            pr, hp = h // 2, (h % 2) * 64
            q0, i = qrange(kt)
            ps = n % 4
            T, nb = kt // 4, kt // 2
            mm(PSB[ps][:, q0:TT], KT[:, pr, kt * 128:(kt + 1) * 128], scr(8 + h)[:, q0:TT], True, False,
               [bKT[pr][T], bSCR[8 + h]], bPS[ps])
            mm(PSB[ps][:, q0:TT], KX[:, nb, :], QX[:, h, q0:TT], False, True, [bKX, bQXc, bQXm[h]], bPS[ps])
            if i >= 0:
                vop(lambda e, ps=ps, q0=q0: e.tensor_tensor(out=PSB[ps][:, q0:q0 + 128], in0=PSB[ps][:, q0:q0 + 128], in1=TRI[:, :], op=ALU.add),
                    [bPS[ps], bTRI], [bPS[ps]])
            col = colbase + kt * NH + h
            pt = 24 + n % 4
            aop(lambda e, ps=ps, q0=q0, col=col, pt=pt: e.activation(out=scr(pt)[:, q0:TT], in_=PSB[ps][:, q0:TT], func=ACT.Exp,
                                                                     bias=ALB[:, col:col + 1], scale=1.0),
                [bPS[ps], bALB], [bSCR[pt]])

        def emit_pv(n):
            h, kt = seq[n]
            q0, i = qrange(kt)
            pt = 24 + n % 4
            acc = 4 + h % 2
            mm(PSB[acc][0:65, q0:TT], VA[:, kt, h, :], scr(pt)[:, q0:TT], kt == first_kt[h], kt == nkt - 1,
               [bVA[kt], bVAones, bSCR[pt]], bPS[acc], sig=True)
            if kt == nkt - 1:
                vop(lambda e, acc=acc: e.reciprocal(out=RR[64:65, :], in_=PSB[acc][64:65, :]), [bPS[acc]], [bRR])
                mm(PSB[6][0:64, :], ONES[:, 0:64], RR[:, :], True, True, [bONES, bRR], bPS[6])
                aop(lambda e: e.copy(out=BCS[:, :], in_=PSB[6][0:64, :]), [bPS[6]], [bBCS])
                vop(lambda e, acc=acc, h=h: e.tensor_tensor(out=scr(h)[0:64, :], in0=PSB[acc][0:64, :], in1=BCS[:, :], op=ALU.mult),
                    [bPS[acc], bBCS], [bSCR[h]])

        for n in range(len(seq) + LAG):
            if n < len(seq):
                emit_s(n)
            if n >= LAG:
                emit_pv(n - LAG)

    def pool_mixer(j, hb_cur, hb_prev):
        vop(lambda e: e.tensor_scalar(out=HUSE[:, :, :], in0=HALO[hb_cur][:, :, :], scalar1=HSEL[:, 2 * j:2 * j + 1], scalar2=None, op0=ALU.mult),
            [bHALO[hb_cur], bHSEL], [bHUSE])
        vop(lambda e: e.scalar_tensor_tensor(out=HUSE[:, :, :], in0=HALO[hb_prev][:, :, :], scalar=HSEL[:, 2 * j + 1:2 * j + 2],
                                             in1=HUSE[:, :, :], op0=ALU.mult, op1=ALU.add),
            [bHALO[hb_prev], bHSEL, bHUSE], [bHUSE])
        PP, TA, TB = FB[:, 0, :], FB[:, 1, :], FB[:, 2, :]
        for g in range(4):
            s, bw = wget("wp", g)
            w3 = wk3(s)
            ps = g % 4
            for k in range(8):
                mm(PSB[ps][:, :], w3[:, k, :], scr(k), k == 0, k == 7, [bw, bSCR[k]], bPS[ps])
            vop(lambda e, ps=ps: e.tensor_copy(out=PP[:, 16:528], in_=PSB[ps][:, :]), [bPS[ps]], [bFB[0]])
            vop(lambda e, g=g: e.tensor_copy(out=PP[:, 0:16], in_=HUSE[:, g, :]), [bHUSE], [bFB[0]])
            src, sb = PP, bFB[0]
            dsts = [(TA, bFB[1]), (TB, bFB[2])]
            sh = 1
            for step in range(g + 1):
                dst, db = dsts[step % 2]
                lo = 2 * sh - 1
                vop(lambda e, dst=dst, src=src, lo=lo, sh=sh: e.tensor_tensor(out=dst[:, lo:528], in0=src[:, lo:528], in1=src[:, lo - sh:528 - sh], op=ALU.add),
                    [sb], [db])
                src, sb = dst, db
                sh *= 2
            w = 2 ** (g + 1)
            vop(lambda e, g=g, src=src, w=w: e.scalar_tensor_tensor(out=scr(16 + g), in0=src[:, 16:528], scalar=1.0 / w, in1=PP[:, 16:528],
                                                                    op0=ALU.mult, op1=ALU.subtract),
                [sb, bFB[0]], [bSCR[16 + g]])
            if j == 0:
                vop(lambda e, g=g, src=src: e.tensor_tensor(out=D16[:, :], in0=src[:, 16:32], in1=INV0[:, g, :], op=ALU.mult),
                    [sb, bINV0], [bD16])
                vop(lambda e, g=g: e.tensor_tensor(out=scr(16 + g)[:, 0:16], in0=D16[:, :], in1=PP[:, 16:32], op=ALU.subtract),
                    [bD16, bFB[0]], [bSCR[16 + g]])
        for g in range(4):
            ps = g % 4
            mm(PSB[ps][:, :], PW[:, g, :], scr(16 + g), True, True, [bPW, bSCR[16 + g]], bPS[ps])
            vop(lambda e, g=g, ps=ps: e.tensor_scalar(out=scr(20 + g), in0=PSB[ps][:, :], scalar1=GA[:, G_PSC + g:G_PSC + g + 1], scalar2=None, op0=ALU.mult),
                [bPS[ps], bGA], [bSCR[20 + g]])

    def mix_out(xs):
        for m in range(8):
            sa, ba = wget("woa", m)
            sp, bp = wget("wop", m)
            wa3 = WS[0:64, sa, :].rearrange("p (h f) -> p h f", h=NH)
            wp3 = WS[:, sp, 0:512].rearrange("p (g f) -> p g f", g=4)
            ps = m % 4
            for h in range(NH):
                mm(PSB[ps][:, :], wa3[:, h, :], scr(h)[0:64, :], h == 0, False, [ba, bSCR[h]], bPS[ps])
            for g in range(4):
                mm(PSB[ps][:, :], wp3[:, g, :], scr(20 + g), False, g == 3, [bp, bSCR[20 + g]], bPS[ps])
            evac_for_postnorm(m, ps, 8)
        postnorm_residual(xs, G_MIXPOST, 8)

    def xa_setup():
        MT = FB[:, :, 0:256]
        S.dma("sync", "memld", lambda e: e.dma_start(out=MT, in_=memT), writes=bFB)
        for k in range(8):
            aop(lambda e, k=k: e.activation(out=scr(8 + k)[:, 0:256], in_=FB[:, k, 0:256], func=ACT.Square), [bFB[k]], [bSCR[8 + k]])
        for k in range(8):
            mm(PSB[6][:, 0:256], ONES[:, :], scr(8 + k)[:, 0:256], k == 0, k == 7, [bONES, bSCR[8 + k]], bPS[6])
        aop(lambda e: e.activation(out=TMP[:, 0, 0:256], in_=PSB[6][:, 0:256], func=ACT.Sqrt, bias=EPST[:, 0:1], scale=1.0 / D),
            [bPS[6], bEPS], [bTMP[0]])
        vop(lambda e: e.reciprocal(out=RT[:, 0:256], in_=TMP[:, 0, 0:256]), [bTMP[0]], [bRT])
        for k in range(8):
            vop(lambda e, k=k: e.scalar_tensor_tensor(out=scr(k)[:, 0:256], in0=FB[:, k, 0:256], scalar=GA[:, G_MEM + k:G_MEM + k + 1],
                                                      in1=RT[:, 0:256], op0=ALU.mult, op1=ALU.mult),
                [bFB[k], bGA, bRT], [bSCR[k]])
        for c in range(8):
            s, bw = wget("xk", c)
            w3 = wk3(s)
            ps = c % 4
            for k in range(8):
                mm(PSB[ps][:, 0:256], w3[:, k, :], scr(k)[:, 0:256], k == 0, k == 7, [bw, bSCR[k]], bPS[ps])
            aop(lambda e, c=c, ps=ps: e.copy(out=KMX[:, c, :], in_=PSB[ps][:, 0:256]), [bPS[ps]], [bKMX[c]])
        for c in range(8):
            s, bw = wget("xv", c)
            w3 = wk3(s)
            for mt in range(2):
                ps = 4 + mt
                for k in range(8):
                    mm(PSB[ps][:, 0:128], scr(k)[:, mt * 128:(mt + 1) * 128], w3[:, k, :], k == 0, k == 7, [bw, bSCR[k]], bPS[ps])
                aop(lambda e, c=c, mt=mt, ps=ps: e.copy(out=VMX[:, mt, c * 128:(c + 1) * 128], in_=PSB[ps][:, 0:128]), [bPS[ps]], [bVMX[mt]])

    def xattn(xs):
        prenorm(xs, G_XAPRE)
        for c in range(8):
            s, bw = wget("xq", c)
            w3 = wk3(s)
            ps = c % 4
            for k in range(8):
                mm(PSB[ps][:, :], w3[:, k, :], scr(k), k == 0, k == 7, [bw, bSCR[k]], bPS[ps])
            aop(lambda e, c=c, ps=ps: e.mul(out=scr(8 + c), in_=PSB[ps][:, :], mul=1.0 / 16), [bPS[ps]], [bSCR[8 + c]])
        for hx in range(4):
            for mt in range(2):
                ps = (2 * hx + mt) % 4
                for cc in range(2):
                    c = 2 * hx + cc
                    mm(PSB[ps][:, :], KMX[:, c, mt * 128:(mt + 1) * 128], scr(8 + c), cc == 0, cc == 1, [bKMX[c], bSCR[8 + c]], bPS[ps])
                aop(lambda e, hx=hx, mt=mt, ps=ps: e.activation(out=scr(16 + 2 * hx + mt), in_=PSB[ps][:, :], func=ACT.Exp),
                    [bPS[ps]], [bSCR[16 + 2 * hx + mt]])
            for mt in range(2):
                mm(PSB[6][:, :], ONES[:, :], scr(16 + 2 * hx + mt), mt == 0, mt == 1, [bONES, bSCR[16 + 2 * hx + mt]], bPS[6])
            vop(lambda e: e.reciprocal(out=RT[:, :], in_=PSB[6][:, :]), [bPS[6]], [bRT])
            for cc in range(2):
                c = 2 * hx + cc
                ps = 4 + cc
                for mt in range(2):
                    mm(PSB[ps][:, :], VMX[:, mt, c * 128:(c + 1) * 128], scr(16 + 2 * hx + mt), mt == 0, mt == 1,
                       [bVMX[mt], bSCR[16 + 2 * hx + mt]], bPS[ps])
                vop(lambda e, c=c, ps=ps: e.tensor_tensor(out=scr(c), in0=PSB[ps][:, :], in1=RT[:, :], op=ALU.mult),
                    [bPS[ps], bRT], [bSCR[c]])
        for m in range(8):
            s, bw = wget("xo", m)
            w3 = wk3(s)
            ps = m % 4
            for k in range(8):
                mm(PSB[ps][:, :], w3[:, k, :], scr(k), k == 0, k == 7, [bw, bSCR[k]], bPS[ps])
            evac_for_postnorm(m, ps, 8)
        postnorm_residual(xs, G_XAPOST, 8)

    def load_x(T, xs):
        for k in range(8):
            S.dma("sync", "x%d_%d" % (xs, k), lambda e, k=k: e.dma_start(out=XS[xs][:, k, :], in_=xT[T, :, k, :]), writes=[bXS[xs][k]])

    s_pw, b_pw = None, None
    S.dma("sync", "pwld", lambda e: e.dma_start(out=PW[:, :, :], in_=w_sc_d["pw"][0].rearrange("p (g f) -> p g f", g=4)),
          reads=[w_sc_buf["pw"][0]], writes=[bPW])
    out_toks = []

    def store_out(j):
        for k in range(8):
            out_toks.append(S.dma("sync", "o%d" % k, lambda e, k=k, j=j: e.dma_start(out=outT[j, :, k, :], in_=XS[1][:, k, :]),
                                  reads=[bXS[1][k]]))

    def xs_items(xs):
        return [(XS[xs][:, k, :], [bXS[xs][k]], 128, TT) for k in range(8)]

    def scr_items(idx, P=128):
        return [(scr(i)[0:P, :], [bSCR[i]], P, TT) for i in idx]

    def main_body():
        load_x(0, 0)
        load_x(1, 1)
        checkpoint("xload", xs_items(0))
        for j in range(4):
            T_other, T_own = 2 * j, 2 * j + 1

            def hook(j=j, T_own=T_own):
                if j > 0:
                    store_out(j - 1)
                    load_x(T_own, 1)

            if j == 0 and stage == "prenorm":
                prenorm(0, G_F1PRE)
                checkpoint("prenorm", scr_items(range(8)))
            ffn(0, "g1", "u1", "d1", G_F1PRE, G_F1POST, mid_hook=hook)
            if j == 0:
                checkpoint("ffn1", xs_items(0))
            prenorm(0, G_MIXPRE)
            proj_k(T_other)
            proj_v(T_other)
            proj_p_halo(j % 2)
            if j == 0:
                checkpoint("kv0", [(KT[:, pr, 0:TT], [bKT[pr][0]], 128, TT) for pr in range(4)]
                           + [(VA[:, i, :, 0:64], [bVA[i]], 128, TT) for i in range(4)])
            ffn(1, "g1", "u1", "d1", G_F1PRE, G_F1POST)
            if j < 3:
                load_x(T_other + 2, 0)
            prenorm(1, G_MIXPRE)
            proj_k(T_own)
            proj_q()
            if j == 0:
                checkpoint("q0", scr_items(range(8, 16)))
            moba_mask(j)
            proj_v(T_own)
            if j == 0:
                checkpoint("mask0", [(QX[0:18, h, :], [bQXm[h], bQXc], 18, TT) for h in range(8)])
            pool_mixer(j, j % 2, (j + 1) % 2)
            if j == 0:
                checkpoint("pool0", scr_items(range(20, 24)))
            attention(j)
            if j == 0:
                checkpoint("attn0", scr_items(range(8), 64))
            if j == 1:
                checkpoint("attn1", scr_items(range(8), 64))
            mix_out(1)
            if j == 0:
                checkpoint("mix0", xs_items(1))
                xa_setup()
            xattn(1)
            if j == 0:
                checkpoint("xa0", xs_items(1))
            ffn(1, "g2", "u2", "d2", G_F2PRE, G_F2POST)
        store_out(3)

    try:
        main_body()
    except _Stop:
        pass
    S.final_wait("sync", out_toks)
    S.final_wait("gpsimd", dbg_toks)
    S.emit()
    return nc


def _chunked(w):
    n = w.shape[1]
    return np.ascontiguousarray(w.reshape(8, 128, n // 128, 128).transpose(2, 1, 0, 3)).reshape(n // 128, 128, 1024)


def _gcols(v):
    return np.ascontiguousarray(v.reshape(-1, 128).T)


def _const_tables(p):
    slopes = (2.0 ** (-8.0 * np.arange(1, NH + 1) / NH)).astype(np.float64)
    true_tile = lambda T: 2 * (T // 2) + ((1 - p) if T % 2 == 0 else p)
    kj = np.arange(128)
    alb = np.zeros((128, 640), np.float64)
    col = 0
    for j in range(4):
        qbase = TT * (2 * j + p)
        for kt in range(8 * (j + 1)):
            kbase = TT * true_tile(kt // 4) + 128 * (kt % 4)
            for h in range(NH):
                if kbase >= qbase + TT:
                    alb[:, col] = NEG
                else:
                    alb[:, col] = slopes[h] * (kbase + kj - qbase)
                col += 1
    vb = np.zeros((8, 16), np.float64)
    for j in range(4):
        for half in range(2):
            own_true_blk = 2 * (2 * j + p) + half
            for nb in range(16):
                tb = 2 * true_tile(nb // 2) + nb % 2
                if nb // 2 > 2 * j + 1:
                    v = -2e30
                elif tb == own_true_blk:
                    v = 1e30
                elif tb < own_true_blk:
                    v = 0.0
                else:
                    v = -2e30
                vb[2 * j + half, nb] = v
    vbt = np.broadcast_to(np.tile(vb[:, None, :], (1, NH, 1)).reshape(1, 8 * 128), (128, 8 * 128))
    tri = np.where(kj[:, None] <= kj[None, :], 0.0, NEG)
    identn = NEG * np.eye(128)
    q = np.arange(TT)
    qxc = np.zeros((2, NH, TT), np.float64)
    for h in range(NH):
        qxc[0, h] = -slopes[h] * 256 * (q // 256)
        qxc[1, h] = -slopes[h] * (q % 256)
    kx = np.zeros((18, 16, 128), np.float64)
    for nb in range(16):
        kx[nb, nb, :] = 1.0
    kx[16:18] = 1.0
    inv0 = np.zeros((4, 16), np.float64)
    for g in range(4):
        w = 2 ** (g + 1)
        for t in range(16):
            inv0[g, t] = 1.0 / (min(t + 1, w) if p == 0 else w)
    hsel = np.zeros(8, np.float64)
    for j in range(4):
        if p == 1:
            hsel[2 * j] = 1.0
        elif j > 0:
            hsel[2 * j + 1] = 1.0
    f = lambda a: np.ascontiguousarray(a, dtype=np.float32)
    return {
        "albias": f(alb), "vb": f(vbt), "tri": f(tri), "identn": f(identn), "qxc": f(qxc.reshape(2, NH * TT)),
        "kx": f(kx.reshape(18, 16 * 128)), "inv0": f(np.broadcast_to(inv0.reshape(1, 64), (128, 64))),
        "hsel": f(np.broadcast_to(hsel[None, :], (128, 8))),
    }


_NC_CACHE = {}
_DEV = {}


def kernel(x, mem, ffn1_pre_g, ffn1_w_gate, ffn1_w_up, ffn1_w_down, ffn1_post_g,
           mix_pre_g, w_in, pool_w, pool_scale, w_out, mix_post_g,
           xa_pre_g, mem_g, xa_wq, xa_wkv, xa_wo, xa_post_g,
           ffn2_pre_g, ffn2_w_gate, ffn2_w_up, ffn2_w_down, ffn2_post_g):
    a = lambda t: np.asarray(t, dtype=np.float32)
    x, mem = a(x), a(mem)
    w_in0, w_out0, xa_wkv0 = a(w_in)[0], a(w_out)[0], a(xa_wkv)[0]
    W = {
        "g1": _chunked(a(ffn1_w_gate)[0]), "u1": _chunked(a(ffn1_w_up)[0]), "d1": np.ascontiguousarray(a(ffn1_w_down)[0].reshape(NFC, 128, 1024)),
        "g2": _chunked(a(ffn2_w_gate)[0]), "u2": _chunked(a(ffn2_w_up)[0]), "d2": np.ascontiguousarray(a(ffn2_w_down)[0].reshape(NFC, 128, 1024)),
        "wq": _chunked(w_in0[:, 0:512]), "wk": _chunked(w_in0[:, 512:1024]), "wv": _chunked(w_in0[:, 1024:1536]),
        "wp": _chunked(w_in0[:, 1536:2048]),
        "woa": np.ascontiguousarray(w_out0[:512].reshape(8, 64, 8, 128).transpose(2, 1, 0, 3)).reshape(8, 64, 1024),
        "wop": np.ascontiguousarray(w_out0[512:].reshape(4, 128, 8, 128).transpose(2, 1, 0, 3)).reshape(8, 128, 512),
        "xq": _chunked(a(xa_wq)[0]), "xk": _chunked(xa_wkv0[:, :1024]), "xv": _chunked(xa_wkv0[:, 1024:]),
        "xo": _chunked(a(xa_wo)[0]),
        "pw": np.ascontiguousarray(a(pool_w)[0].transpose(1, 0, 2)).reshape(1, 128, 512),
    }
    gains = np.concatenate([_gcols(a(g)[0]) for g in (ffn1_pre_g, ffn1_post_g, mix_pre_g, mix_post_g, xa_pre_g, mem_g,
                                                       xa_post_g, ffn2_pre_g, ffn2_post_g, pool_scale)], axis=1)
    gains = np.ascontiguousarray(gains, dtype=np.float32)
    assert gains.shape == (128, NGCOL)
    tabs = [_const_tables(0), _const_tables(1)]
    in_maps = []
    for c in range(8):
        b, p = c // 2, c % 2
        order = []
        for j in range(4):
            order += [2 * j + 1 - p, 2 * j + p]
        xb = x[b].reshape(8, TT, 8, 128)
        xTc = np.ascontiguousarray(xb[order].transpose(0, 3, 2, 1))
        memTc = np.ascontiguousarray(mem[b].reshape(256, 8, 128).transpose(2, 1, 0))
        m = {"xT": xTc, "memT": memTc, "gains": gains}
        m.update(tabs[p])
        for fam, arr in W.items():
            m["w_" + fam] = arr
        in_maps.append(m)
    if _DEV.get("stage"):
        return in_maps
    if "nc" not in _NC_CACHE:
        _NC_CACHE["nc"] = build_program()
    res = run_bass_kernel_spmd(_NC_CACHE["nc"], in_maps, core_ids=list(range(8)))
    out = np.empty((NBATCH, SEQ, D), np.float32)
    for c in range(8):
        b, p = c // 2, c % 2
        o = np.asarray(res.results[c]["outT"])
        for j in range(4):
            t = 2 * j + p
            out[b, t * TT:(t + 1) * TT, :] = o[j].transpose(2, 1, 0).reshape(TT, D)
    return out
```
